# Optimizing a Trainium2 kernel written in Bass

```python
import math
import jax
import jax.numpy as jnp
from jax import lax
import numpy as np

D_MODEL = 2048
BATCH = 2
SEQ = 4096
DEPTH = 4

GRID_W = 64
CTX_LEN = 256
ROPE_THETA = 10000.0
NORM_EPS = 1e-6
NEG_INF = -1e30
Q_BLOCK = 128

MLA_H = 4
MLA_Q_LORA = 512
MLA_KV_LORA = 256
MLA_NOPE = 128
MLA_ROPE = 64
MLA_V = 128
SWA_H = 4
SWA_KV_H = 2
SWA_HD = 128
SWA_WINDOW = 128
SWA_BLOCK = 128
NA_H = 4
NA_HD = 128
NA_KH = 8
NA_KW = 16
DIFF_H = 4
DIFF_DK = 64
DIFF_DV = 128
N_BRANCH = 4
BRANCH_W = 512
D_FF = 5632
N_EXPERTS = 8
TOP_K = 2
D_FF_EXPERT = 5632
N_DENSE = (DEPTH + 1) // 2
N_MOE = DEPTH // 2

IN_SIZES = (
    MLA_Q_LORA, MLA_KV_LORA, MLA_ROPE,
    SWA_H * SWA_HD, SWA_KV_H * SWA_HD, SWA_KV_H * SWA_HD,
    NA_H * NA_HD, NA_H * NA_HD, NA_H * NA_HD,
    DIFF_H * 2 * DIFF_DK, DIFF_H * 2 * DIFF_DK, DIFF_H * DIFF_DV,
    N_BRANCH * D_MODEL,
)
IN_WIDTH = sum(IN_SIZES)

kernel_name = "hybrid_gated_branch_flow_backbone"


def rmsnorm(x, g):
    xf = x.astype(jnp.float32)
    y = xf * lax.rsqrt(jnp.mean(xf * xf, axis=-1, keepdims=True) + NORM_EPS)
    return (y * g.astype(jnp.float32)).astype(x.dtype)


def modulate(x, shift, scale):
    return x * (1 + scale) + shift


def split_in(u):
    parts, start = [], 0
    for width in IN_SIZES:
        parts.append(u[..., start:start + width])
        start += width
    return parts


def heads(t, n_h, d):
    return t.reshape(t.shape[0], t.shape[1], n_h, d)


def cat_seq(a, b):
    return jnp.concatenate([a, b], axis=1)


def rope_tables(n, dim):
    pos = jnp.arange(n, dtype=jnp.int32)
    row = (pos // GRID_W).astype(jnp.float32)
    col = (pos % GRID_W).astype(jnp.float32)
    quarter = dim // 4
    inv_freq = ROPE_THETA ** (-jnp.arange(quarter, dtype=jnp.float32) / quarter)
    ang_r = row[:, None] * inv_freq
    ang_c = col[:, None] * inv_freq
    ang = jnp.concatenate([ang_r, ang_r, ang_c, ang_c], axis=-1)
    return jnp.cos(ang), jnp.sin(ang)


def axial_rope(x, cos, sin):
    half = x.shape[-1] // 2
    quarter = half // 2

    def rot(y):
        return jnp.concatenate([-y[..., quarter:], y[..., :quarter]], axis=-1)

    x_rot = jnp.concatenate([rot(x[..., :half]), rot(x[..., half:])], axis=-1)
    return x * cos[:, None, :].astype(x.dtype) + x_rot * sin[:, None, :].astype(x.dtype)


def _identity(p):
    return p


def dense_block_attention(q, k, v, combine):
    bsz, n_q, n_g, dk = q.shape
    n_blk = n_q // Q_BLOCK
    scale = dk ** -0.5
    q_blocks = q.reshape(bsz, n_blk, Q_BLOCK, n_g, dk).transpose(1, 0, 2, 3, 4)

    def one_block(qb):
        s = jnp.einsum("bqgd,bngd->bgqn", qb, k, preferred_element_type=jnp.float32) * scale
        w = combine(jax.nn.softmax(s, axis=-1)).astype(v.dtype)
        return jnp.einsum("bhqn,bnhd->bqhd", w, v)

    o = lax.map(one_block, q_blocks)
    return o.transpose(1, 0, 2, 3, 4).reshape(bsz, n_q, v.shape[2], v.shape[3])


def sink_probs(s, sink):
    m = jnp.maximum(jnp.max(s, axis=-1, keepdims=True), sink)
    e = jnp.exp(s - m)
    return e / (jnp.sum(e, axis=-1, keepdims=True) + jnp.exp(sink - m))


def mla_queries(cq, q_norm_g, w_uq, rope):
    bsz, n, _ = cq.shape
    q = (rmsnorm(cq, q_norm_g) @ w_uq).reshape(bsz, n, MLA_H, MLA_NOPE + MLA_ROPE)
    if rope is None:
        return q
    return jnp.concatenate([q[..., :MLA_NOPE], axial_rope(q[..., MLA_NOPE:], *rope)], axis=-1)


def mla_keys_values(ckv, k_rope, kv_norm_g, w_ukv, rope):
    bsz, n, _ = ckv.shape
    kv = (rmsnorm(ckv, kv_norm_g) @ w_ukv).reshape(bsz, n, MLA_H, MLA_NOPE + MLA_V)
    kr = k_rope[:, :, None, :]
    if rope is not None:
        kr = axial_rope(kr, *rope)
    k = jnp.concatenate([kv[..., :MLA_NOPE], jnp.broadcast_to(kr, (bsz, n, MLA_H, MLA_ROPE))], axis=-1)
    return k, kv[..., MLA_NOPE:]


def window_gqa_attention(q, k, v, k_ctx, v_ctx, sink):
    bsz, n, n_h, d = q.shape
    n_kv = k.shape[2]
    grp = n_h // n_kv
    blk = SWA_BLOCK
    n_blk = n // blk

    def bands(t):
        tp = jnp.pad(t, ((0, 0), (blk, blk), (0, 0), (0, 0))).reshape(bsz, n_blk + 2, blk, n_kv, d)
        return jnp.concatenate([tp[:, :-2], tp[:, 1:-1], tp[:, 2:]], axis=2)

    kb, vb = bands(k), bands(v)
    qb = q.reshape(bsz, n_blk, blk, n_kv, grp, d)
    scale = d ** -0.5
    s_loc = jnp.einsum("bnqkgd,bnjkd->bnkgqj", qb, kb, preferred_element_type=jnp.float32) * scale
    s_ctx = jnp.einsum("bnqkgd,bckd->bnkgqc", qb, k_ctx, preferred_element_type=jnp.float32) * scale
    qi = jnp.arange(blk)[:, None]
    jj = jnp.arange(3 * blk)[None, :]
    key_pos = jnp.arange(n_blk)[:, None, None] * blk - blk + jj[None]
    valid = (jnp.abs(jj - blk - qi)[None] <= SWA_WINDOW) & (key_pos >= 0) & (key_pos < n)
    s_loc = jnp.where(valid[None, :, None, None], s_loc, NEG_INF)
    sink_b = sink.astype(jnp.float32).reshape(n_kv, grp)[None, None, :, :, None, None]
    p = sink_probs(jnp.concatenate([s_ctx, s_loc], axis=-1), sink_b).astype(v.dtype)
    n_ctx = k_ctx.shape[1]
    o = (jnp.einsum("bnkgqc,bckd->bnqkgd", p[..., :n_ctx], v_ctx)
         + jnp.einsum("bnkgqj,bnjkd->bnqkgd", p[..., n_ctx:], vb))
    return o.reshape(bsz, n, n_h, d)


def context_sink_attention(q, k, v, sink):
    bsz, n, n_h, d = q.shape
    n_kv = k.shape[2]
    grp = n_h // n_kv
    qg = q.reshape(bsz, n, n_kv, grp, d)
    s = jnp.einsum("bqkgd,bckd->bkgqc", qg, k, preferred_element_type=jnp.float32) * d ** -0.5
    p = sink_probs(s, sink.astype(jnp.float32).reshape(n_kv, grp)[None, :, :, None, None])
    o = jnp.einsum("bkgqc,bckd->bqkgd", p.astype(v.dtype), v)
    return o.reshape(bsz, n, n_h, d)


def neighbourhood_attention(q, k, v, k_ctx, v_ctx, rpb):
    bsz, n, n_h, d = q.shape
    rows = n // GRID_W
    kh = min(NA_KH, rows)
    kw = NA_KW
    r = jnp.arange(rows)
    row_idx = jnp.clip(r - kh // 2, 0, rows - kh)[:, None] + jnp.arange(kh)[None, :]
    col = jnp.arange(GRID_W)
    col_start = jnp.clip(col - kw // 2, 0, GRID_W - kw)
    col_valid = (col[None, :] >= col_start[:, None]) & (col[None, :] < col_start[:, None] + kw)
    dr = row_idx - r[:, None] + (NA_KH - 1)
    dc = jnp.clip(col[None, :] - col[:, None], -(kw - 1), kw - 1) + (kw - 1)
    bias = rpb[:, dr][..., dc]
    bias = bias.transpose(1, 0, 3, 2, 4).reshape(rows, n_h, GRID_W, kh * GRID_W).astype(jnp.float32)
    valid = jnp.broadcast_to(col_valid[:, None, :], (GRID_W, kh, GRID_W)).reshape(GRID_W, kh * GRID_W)
    qg = q.reshape(bsz, rows, GRID_W, n_h, d)
    kg = k.reshape(bsz, rows, GRID_W, n_h, d)[:, row_idx].reshape(bsz, rows, kh * GRID_W, n_h, d)
    vg = v.reshape(bsz, rows, GRID_W, n_h, d)[:, row_idx].reshape(bsz, rows, kh * GRID_W, n_h, d)
    scale = d ** -0.5
    s_loc = jnp.einsum("brqhd,brkhd->brhqk", qg, kg, preferred_element_type=jnp.float32) * scale + bias[None]
    s_loc = jnp.where(valid, s_loc, NEG_INF)
    s_ctx = jnp.einsum("brqhd,bchd->brhqc", qg, k_ctx, preferred_element_type=jnp.float32) * scale
    p = jax.nn.softmax(jnp.concatenate([s_ctx, s_loc], axis=-1), axis=-1).astype(v.dtype)
    n_ctx = k_ctx.shape[1]
    o = (jnp.einsum("brhqc,bchd->brqhd", p[..., :n_ctx], v_ctx)
         + jnp.einsum("brhqk,brkhd->brqhd", p[..., n_ctx:], vg))
    return o.reshape(bsz, n, n_h, d)


def diff_qk(t, rope):
    bsz, n, _ = t.shape
    t = t.reshape(bsz, n, 2 * DIFF_H, DIFF_DK)
    return t if rope is None else axial_rope(t, *rope)


def merge_branches(outs, gate_raw, w_branch, w_out):
    bsz, n = gate_raw.shape[:2]
    gates = jax.nn.sigmoid(gate_raw.reshape(bsz, n, N_BRANCH, D_MODEL))
    merged = None
    for m, o in enumerate(outs):
        y = gates[:, :, m] * (o.reshape(bsz, n, BRANCH_W) @ w_branch[m])
        merged = y if merged is None else merged + y
    return merged @ w_out


def token_mixer(u_lat, u_ctx, p, ropes, lam_init, need_ctx):
    L = split_in(u_lat)
    X = split_in(u_ctx)
    lat_out, ctx_out = [], []

    k_c, v_c = mla_keys_values(X[1], X[2], p["mla_kv_norm"], p["mla_w_ukv"], None)
    k_l, v_l = mla_keys_values(L[1], L[2], p["mla_kv_norm"], p["mla_w_ukv"], ropes[MLA_ROPE])
    q_l = mla_queries(L[0], p["mla_q_norm"], p["mla_w_uq"], ropes[MLA_ROPE])
    lat_out.append(dense_block_attention(q_l, cat_seq(k_c, k_l), cat_seq(v_c, v_l), _identity))
    if need_ctx:
        q_c = mla_queries(X[0], p["mla_q_norm"], p["mla_w_uq"], None)
        ctx_out.append(dense_block_attention(q_c, k_c, v_c, _identity))

    rope_b = ropes[SWA_HD]
    q_l = axial_rope(heads(L[3], SWA_H, SWA_HD), *rope_b)
    k_l = axial_rope(heads(L[4], SWA_KV_H, SWA_HD), *rope_b)
    v_l = heads(L[5], SWA_KV_H, SWA_HD)
    k_c = heads(X[4], SWA_KV_H, SWA_HD)
    v_c = heads(X[5], SWA_KV_H, SWA_HD)
    lat_out.append(window_gqa_attention(q_l, k_l, v_l, k_c, v_c, p["swa_sink"]))
    if need_ctx:
        ctx_out.append(context_sink_attention(heads(X[3], SWA_H, SWA_HD), k_c, v_c, p["swa_sink"]))

    k_c = heads(X[7], NA_H, NA_HD)
    v_c = heads(X[8], NA_H, NA_HD)
    lat_out.append(neighbourhood_attention(heads(L[6], NA_H, NA_HD), heads(L[7], NA_H, NA_HD),
                                           heads(L[8], NA_H, NA_HD), k_c, v_c, p["na_rpb"]))
    if need_ctx:
        ctx_out.append(dense_block_attention(heads(X[6], NA_H, NA_HD), k_c, v_c, _identity))

    lp = p["diff_lambda"].astype(jnp.float32)
    lam = jnp.exp(jnp.sum(lp[0] * lp[1])) - jnp.exp(jnp.sum(lp[2] * lp[3])) + lam_init

    def diff_combine(pr):
        pr = pr.reshape(pr.shape[0], DIFF_H, 2, pr.shape[2], pr.shape[3])
        return pr[:, :, 0] - lam * pr[:, :, 1]

    def diff_post(o):
        return rmsnorm(o, p["diff_subln_g"]) * (1.0 - lam_init)

    k_c = diff_qk(X[10], None)
    v_c = heads(X[11], DIFF_H, DIFF_DV)
    k_l = diff_qk(L[10], ropes[DIFF_DK])
    v_l = heads(L[11], DIFF_H, DIFF_DV)
    lat_out.append(diff_post(dense_block_attention(diff_qk(L[9], ropes[DIFF_DK]), cat_seq(k_c, k_l),
                                                   cat_seq(v_c, v_l), diff_combine)))
    if need_ctx:
        ctx_out.append(diff_post(dense_block_attention(diff_qk(X[9], None), k_c, v_c, diff_combine)))

    y_lat = merge_branches(lat_out, L[12], p["w_branch"], p["w_out"])
    y_ctx = merge_branches(ctx_out, X[12], p["w_branch"], p["w_out"]) if need_ctx else None
    return y_lat, y_ctx


def swiglu(x, w1, w3, w2):
    return (jax.nn.silu(x @ w1) * (x @ w3)) @ w2


def moe_swiglu(x, router, w1, w3, w2):
    logits = (x @ router).astype(jnp.float32)
    top_v, top_i = lax.top_k(logits, TOP_K)
    top_g = jax.nn.softmax(top_v, axis=-1)
    gate = jnp.sum(jax.nn.one_hot(top_i, N_EXPERTS, dtype=jnp.float32) * top_g[..., None], axis=-2)
    out = None
    for e in range(N_EXPERTS):
        y = gate[..., e:e + 1].astype(x.dtype) * swiglu(x, w1[e], w3[e], w2[e])
        out = y if out is None else out + y
    return out


def setup_inputs(seed: int = 0) -> dict:
    key = jax.random.key(seed)
    keys = iter(jax.random.split(key, 32))

    def nrm(shape, scale):
        return jax.random.normal(next(keys), shape, jnp.float32) * scale

    L, D = DEPTH, D_MODEL
    return {
        "x": nrm((BATCH, SEQ, D), 1.0),
        "c": nrm((BATCH, D), 1.0),
        "ctx": nrm((BATCH, CTX_LEN, D), 1.0),
        "c_ctx": nrm((D,), 1.0),
        "mod_w": nrm((L, D, 6 * D), 0.5 * D ** -0.5),
        "mod_b": nrm((L, 6 * D), 0.02),
        "norm1_g": 1.0 + nrm((L, D), 0.05),
        "norm2_g": 1.0 + nrm((L, D), 0.05),
        "w_in": nrm((L, D, IN_WIDTH), D ** -0.5),
        "mla_q_norm": 1.0 + nrm((L, MLA_Q_LORA), 0.05),
        "mla_kv_norm": 1.0 + nrm((L, MLA_KV_LORA), 0.05),
        "mla_w_uq": nrm((L, MLA_Q_LORA, MLA_H * (MLA_NOPE + MLA_ROPE)), MLA_Q_LORA ** -0.5),
        "mla_w_ukv": nrm((L, MLA_KV_LORA, MLA_H * (MLA_NOPE + MLA_V)), MLA_KV_LORA ** -0.5),
        "swa_sink": nrm((L, SWA_H), 0.5),
        "na_rpb": nrm((L, NA_H, 2 * NA_KH - 1, 2 * NA_KW - 1), 0.1),
        "diff_lambda": nrm((L, 4, DIFF_DK), 0.1),
        "diff_subln_g": 1.0 + nrm((L, DIFF_DV), 0.05),
        "w_branch": nrm((L, N_BRANCH, BRANCH_W, D), BRANCH_W ** -0.5),
        "w_out": nrm((L, D, D), D ** -0.5),
        "ffn_w1": nrm((N_DENSE, D, D_FF), D ** -0.5),
        "ffn_w3": nrm((N_DENSE, D, D_FF), D ** -0.5),
        "ffn_w2": nrm((N_DENSE, D_FF, D), D_FF ** -0.5),
        "moe_router": nrm((N_MOE, D, N_EXPERTS), D ** -0.5),
        "moe_w1": nrm((N_MOE, N_EXPERTS, D, D_FF_EXPERT), D ** -0.5),
        "moe_w3": nrm((N_MOE, N_EXPERTS, D, D_FF_EXPERT), D ** -0.5),
        "moe_w2": nrm((N_MOE, N_EXPERTS, D_FF_EXPERT, D), D_FF_EXPERT ** -0.5),
        "final_norm_g": 1.0 + nrm((D,), 0.05),
    }


def reference(x, c, ctx, c_ctx, mod_w, mod_b, norm1_g, norm2_g, w_in, mla_q_norm, mla_kv_norm,
              mla_w_uq, mla_w_ukv, swa_sink, na_rpb, diff_lambda, diff_subln_g, w_branch, w_out,
              ffn_w1, ffn_w3, ffn_w2, moe_router, moe_w1, moe_w3, moe_w2, final_norm_g):
    n_lat = x.shape[1]
    ropes = {dim: rope_tables(n_lat, dim) for dim in (MLA_ROPE, SWA_HD, DIFF_DK)}
    h, hc = x, ctx
    for l in range(DEPTH):
        need_ctx = l < DEPTH - 1
        lam_init = 0.8 - 0.6 * math.exp(-0.3 * l)
        mod = jax.nn.silu(c) @ mod_w[l] + mod_b[l]
        mod_c = jax.nn.silu(c_ctx) @ mod_w[l] + mod_b[l]
        sh1, sc1, g1, sh2, sc2, g2 = jnp.split(mod[:, None, :], 6, axis=-1)
        csh1, csc1, cg1, csh2, csc2, cg2 = jnp.split(mod_c, 6, axis=-1)

        u_lat = modulate(rmsnorm(h, norm1_g[l]), sh1, sc1) @ w_in[l]
        u_ctx = modulate(rmsnorm(hc, norm1_g[l]), csh1, csc1) @ w_in[l]
        p = {
            "mla_q_norm": mla_q_norm[l], "mla_kv_norm": mla_kv_norm[l],
            "mla_w_uq": mla_w_uq[l], "mla_w_ukv": mla_w_ukv[l],
            "swa_sink": swa_sink[l], "na_rpb": na_rpb[l],
            "diff_lambda": diff_lambda[l], "diff_subln_g": diff_subln_g[l],
            "w_branch": w_branch[l], "w_out": w_out[l],
        }
        y_lat, y_ctx = token_mixer(u_lat, u_ctx, p, ropes, lam_init, need_ctx)
        h = h + g1 * y_lat
        if need_ctx:
            hc = hc + cg1 * y_ctx

        xn = modulate(rmsnorm(h, norm2_g[l]), sh2, sc2)
        if need_ctx:
            xcn = modulate(rmsnorm(hc, norm2_g[l]), csh2, csc2)
        if l % 2 == 0:
            i = l // 2
            h = h + g2 * swiglu(xn, ffn_w1[i], ffn_w3[i], ffn_w2[i])
            if need_ctx:
                hc = hc + cg2 * swiglu(xcn, ffn_w1[i], ffn_w3[i], ffn_w2[i])
        else:
            i = l // 2
            h = h + g2 * moe_swiglu(xn, moe_router[i], moe_w1[i], moe_w3[i], moe_w2[i])
            if need_ctx:
                hc = hc + cg2 * moe_swiglu(xcn, moe_router[i], moe_w1[i], moe_w3[i], moe_w2[i])
    return rmsnorm(h, final_norm_g)
```

```python
import math
import numpy as np
from contextlib import ExitStack
import concourse.bass as bass
import concourse.mybir as mybir
from concourse.bass_utils import run_bass_kernel_spmd

F32 = mybir.dt.float32
BF16 = mybir.dt.bfloat16
ALU = mybir.AluOpType
AF = mybir.ActivationFunctionType
AX = mybir.AxisListType
P = 128
NCORES = 8
G = 4
RGROUPS = [[0, 1, 2, 3], [4, 5, 6, 7]]
NR = 2
import os
FAKECC = bool(os.environ.get("MK_FAKECC"))
COMPUTE = ("pe", "act", "dve", "pool")


ALL_BUFS = []


class _Stop(Exception):
    pass


class Buf:
    __slots__ = ("name", "w_eng", "w_dma", "r_eng", "r_dma")

    def __init__(self, name=""):
        ALL_BUFS.append(self)
        self.name = name
        self.w_eng = {}
        self.w_dma = []
        self.r_eng = {}
        self.r_dma = []


class Op:
    __slots__ = ("eng", "fn", "deps", "signaled", "sem", "val", "is_dma")

    def __init__(self, eng, fn, is_dma):
        self.eng = eng
        self.fn = fn
        self.deps = []
        self.signaled = False
        self.sem = None
        self.val = 0
        self.is_dma = is_dma


class Sched:
    def __init__(self, nc, n_dma_sems=12, n_cc_sems=64):
        self.nc = nc
        self.ops = {e: [] for e in ("pe", "act", "dve", "pool", "sp")}
        self.n_dma_sems = n_dma_sems
        self.n_cc_sems = n_cc_sems

    def op(self, eng, fn, reads=(), writes=(), adds=(), dma=False, cc=False):
        o = Op(eng, fn, "cc" if cc else bool(dma))
        if cc:
            o.signaled = True
        deps = {}

        def add(d):
            if d is not o:
                deps[id(d)] = d

        for b in reads:
            for d in b.w_eng.values():
                add(d)
            for d in b.w_dma:
                add(d)
        for b in writes:
            for d in b.w_eng.values():
                add(d)
            for d in b.w_dma:
                add(d)
            for d in b.r_eng.values():
                add(d)
            for d in b.r_dma:
                add(d)
        for b in adds:
            for d in b.r_eng.values():
                add(d)
            for d in b.r_dma:
                add(d)
            for e2, d in b.w_eng.items():
                if o.is_dma or e2 != eng:
                    add(d)
            if not o.is_dma:
                for d in b.w_dma:
                    add(d)
        dl = []
        for d in deps.values():
            if (not d.is_dma) and (not o.is_dma) and d.eng == eng == "pe":
                continue
            d.signaled = True
            dl.append(d)
        o.deps = dl
        for b in writes:
            b.w_eng = {}
            b.w_dma = []
            b.r_eng = {}
            b.r_dma = []
        for b in list(writes) + list(adds):
            if o.is_dma:
                b.w_dma.append(o)
            else:
                b.w_eng[eng] = o
        for b in reads:
            if o.is_dma:
                b.r_dma.append(o)
            else:
                b.r_eng[eng] = o
        self.ops[eng].append(o)
        return o

    def emit(self):
        nc = self.nc
        with ExitStack() as st:
            esem = {e: st.enter_context(nc.semaphore(f"s_{e}")) for e in COMPUTE}
            dsem = {q: [st.enter_context(nc.semaphore(f"d_{q}{i}")) for i in range(self.n_dma_sems)]
                    for q in ("sp", "pool")}
            ccsem = st.enter_context(nc.semaphore("ccsem"))
            cci = 0
            for e, lst in self.ops.items():
                cnt = 0
                dcnt = 0
                for o in lst:
                    if not o.signaled:
                        continue
                    if o.is_dma == "cc":
                        cci += 1
                        o.sem = ccsem
                        o.val = cci
                    elif o.is_dma:
                        n = self.n_dma_sems
                        o.sem = dsem[e][dcnt % n]
                        o.val = 16 * (dcnt // n + 1)
                        dcnt += 1
                    else:
                        cnt += 1
                        o.sem = esem[e]
                        o.val = cnt
            import os
            if os.environ.get("MK_VERBOSE"):
                for e, lst in self.ops.items():
                    sig = [o for o in lst if o.signaled]
                    print("ENG", e, "ops", len(lst), "signaled", len(sig), "maxval", max([o.val for o in sig] + [0]), flush=True)
            block = st.enter_context(nc.Block())

            def run(eng_name):
                def body(e):
                    waited = {}
                    for o in self.ops[eng_name]:
                        need = {}
                        for d in o.deps:
                            k = id(d.sem)
                            if k not in need or need[k][1] < d.val:
                                need[k] = (d.sem, d.val)
                        for k, (s, v) in need.items():
                            if waited.get(k, 0) >= v:
                                continue
                            e.wait_ge(s, v)
                            waited[k] = v
                        ins = o.fn(e)
                        if o.signaled:
                            ins.then_inc(o.sem, 16 if o.is_dma is True else 1)
                return body

            block.tensor(run("pe"))
            block.scalar(run("act"))
            block.vector(run("dve"))
            block.gpsimd(run("pool"))
            block.sync(run("sp"))


class Cfg:
    def __init__(self, D=2048, SEQ=4096, CTX=256, DFF=5632, DEPTH=4):
        self.D, self.SEQ, self.CTX, self.DFF, self.DEPTH = D, SEQ, CTX, DFF, DEPTH
        self.GW = 64
        self.TB = SEQ + CTX
        self.T = self.TB
        self.CH = D // G
        assert self.CH % P == 0
        self.NCH = self.CH // P
        self.KD = D // P
        self.FD = ((DFF // G) + P - 1) // P * P
        self.FE = (DFF + P - 1) // P * P
        self.NE = 8
        self.ND = (DEPTH + 1) // 2
        self.NM = DEPTH // 2
        self.NQKV = 17 * P
        self.eps = 1e-6

    def lblocks(self):
        out = []
        t = 0
        while t < self.CTX:
            s = min(512, self.CTX - t)
            out.append((t, s, "ctx"))
            t += s
        while t < self.TB:
            s = min(512, self.TB - t)
            out.append((t, s, "lat"))
            t += s
        return out

    def gblocks(self):
        return [(t0, s, 0 if kind == "lat" else 1) for (t0, s, kind) in self.lblocks()]


def kchunks(K):
    out = []
    k = 0
    while k < K:
        out.append((k, min(P, K - k)))
        k += P
    return out


class Blk:
    def __init__(self, builder, name, rows, dt, blocks):
        self.items = []
        for i, blk in enumerate(blocks):
            t0, size = blk[0], blk[1]
            ap, buf = builder.dram(f"{name}_{i}", [rows, size], dt)
            self.items.append((t0, size, ap, buf))

    def get(self, t):
        for (t0, size, ap, buf) in self.items:
            if t0 <= t < t0 + size:
                return ap, buf, t0
        raise KeyError(t)


def vec_cols(cfg):
    cols = {}
    n = 0

    def add(name, cnt):
        nonlocal n
        cols[name] = n
        n += cnt

    add("n1g", cfg.NCH)
    add("n2g", cfg.NCH)
    add("fng", cfg.NCH)
    add("modb", 6 * cfg.NCH)
    add("qng", 4)
    add("kvng", 2)
    add("sink", 1)
    add("subg", 1)
    add("lam4", 4 * 64)
    add("sel", 16)
    return cols, n


def na_schedule(cfg):
    rows = cfg.SEQ // cfg.GW
    KH, KW, GW = 8, 16, cfg.GW
    kh = min(KH, rows)
    r = np.arange(rows)
    row_start = np.clip(r - kh // 2, 0, rows - kh)
    col = np.arange(GW)
    col_start = np.clip(col - KW // 2, 0, GW - KW)
    nq = cfg.SEQ // 512
    uniq = {}
    masks, drs, dcs = [], [], []
    sched = []
    for Q in range(nq):
        qr = np.arange(8 * Q, 8 * Q + 8)
        kr_lo = row_start[qr].min()
        kr_hi = row_start[qr].max() + kh - 1
        lst = []
        for kt in range(kr_lo // 2, kr_hi // 2 + 1):
            kr = np.array([2 * kt, 2 * kt + 1])
            KR = np.repeat(kr, GW)[:, None]
            KC = np.tile(col, 2)[:, None]
            QR = np.repeat(qr, GW)[None, :]
            QC = np.tile(col, 8)[None, :]
            rv = (KR >= row_start[QR]) & (KR < row_start[QR] + kh) & (KR < rows)
            cv = (KC >= col_start[QC]) & (KC < col_start[QC] + KW)
            m = (rv & cv)
            dr = np.clip(KR - QR + (KH - 1), 0, 2 * KH - 2) + 0 * QC
            dc = np.clip(KC - QC, -(KW - 1), KW - 1) + (KW - 1) + 0 * QR
            if not m.any():
                continue
            key = (m.tobytes(), (dr * m).tobytes(), (dc * m).tobytes())
            if key not in uniq:
                uniq[key] = len(masks)
                masks.append(m.astype(np.float32))
                drs.append(dr.astype(np.int64))
                dcs.append(dc.astype(np.int64))
            lst.append((kt, uniq[key]))
        sched.append(lst)
    return sched, np.stack(masks), np.stack(drs), np.stack(dcs)


def swa_schedule(cfg):
    nq = cfg.SEQ // 512
    nkt = cfg.SEQ // P
    sched = []
    variants = {}
    masks = []
    for Q in range(nq):
        lst = []
        for kt in range(max(0, 4 * Q - 1), min(nkt, 4 * Q + 5)):
            k = kt * P + np.arange(P)[:, None]
            q = Q * 512 + np.arange(512)[None, :]
            m = (np.abs(k - q) <= 128)
            if m.all():
                lst.append((kt, None))
                continue
            key = m.tobytes()
            if key not in variants:
                variants[key] = len(masks)
                masks.append(m.astype(np.float32))
            lst.append((kt, variants[key]))
        sched.append(lst)
    return sched, np.stack(masks)


class Builder:
    WB = 16384
    AB = 11264

    def __init__(self, cfg, debug=(), stop=999):
        self.cfg = cfg
        self.stop = stop
        del ALL_BUFS[:]
        self.debug = set(debug)
        self.nc = bass.Bass("TRN2", target_bir_lowering=False)
        self.S = Sched(self.nc)
        self.st = ExitStack()
        nc = self.nc
        wall, _ = self.sb("wbuf_all", [P, 2 * self.WB], BF16)
        self.wall = wall
        self.wbuf = [(wall[:, i * self.WB:(i + 1) * self.WB], Buf(f"wbuf{i}")) for i in range(2)]
        self.abuf = [self.sb(f"abuf{i}", [P, self.AB], BF16) for i in range(2)]
        self.obuf = [self.sb(f"obuf{i}", [P, 4 * 512], F32) for i in range(2)]
        self.obuf16 = [self.sb(f"obh{i}", [P, 8 * 512], BF16) for i in range(2)]
        self.ps = []
        for i in range(8):
            t = self.st.enter_context(nc.psum_tensor(f"ps{i}", [P, 512], F32))
            self.ps.append((t, Buf(f"ps{i}")))
        self.ps_cnt = {}
        self.cnt = {"w": 0, "a": 0, "o": 0, "oh": 0}
        self.inputs = {}
        self.na_s, self.na_mask, self.na_dr, self.na_dc = na_schedule(cfg)
        self.swa_s, self.swa_mask = swa_schedule(cfg)
        self.vcols, self.NV = vec_cols(cfg)
        self._boff = {}

    def sb(self, name, shape, dt):
        t = self.st.enter_context(self.nc.sbuf_tensor(name, shape, dt))
        return (t, Buf(name))

    def dram(self, name, shape, dt):
        kind = "ExternalOutput" if name in self.debug else "Internal"
        t = self.nc.dram_tensor(name, shape, dt, kind=kind).ap()
        return (t, Buf(name))

    def inp(self, name, shape, dt=F32):
        t = self.nc.dram_tensor(name, shape, dt, kind="ExternalInput").ap()
        self.inputs[name] = (tuple(shape), dt)
        return (t, Buf(name))

    def psum(self, pool=(0, 1, 2, 3, 4, 5, 6, 7)):
        c = self.ps_cnt.get(pool, 0)
        self.ps_cnt[pool] = c + 1
        return self.ps[pool[c % len(pool)]]

    def rot(self, which, lst):
        r = lst[self.cnt[which] % len(lst)]
        self.cnt[which] += 1
        return r

    def A(self, eng, fn, r=(), w=(), a=(), dma=False, cc=False):
        return self.S.op(eng, fn, reads=r, writes=w, adds=a, dma=dma, cc=cc)

    def dma(self, out, in_, r, w=(), a=(), q="sp"):
        return self.A(q, lambda e: e.dma_start(out=out, in_=in_), r, w, a, dma=True)

    def boff(self, e, key):
        if key not in self._boff:
            self._boff[key] = (e.partition_id() // 4) * self.cfg.TB
        return self._boff[key]

    def load_weights(self, W, m0, mg, kch, dst=None, combined=False):
        Wap, Wbuf = W
        nkc = len(kch)
        if combined:
            assert nkc * mg <= 2 * self.WB, (nkc, mg)
            view = self.wall[:, 0:nkc * mg].rearrange("p (k m) -> p k m", m=mg)
            wbl = [self.wbuf[0][1], self.wbuf[1][1]]
        elif dst is None:
            wt, wb = self.rot("w", self.wbuf)
            assert nkc * mg <= self.WB, (nkc, mg)
            view = wt[:, 0:nkc * mg].rearrange("p (k m) -> p k m", m=mg)
            wbl = [wb]
        else:
            view, wb = dst
            wbl = [wb]
        nfull = sum(1 for (_, ks) in kch if ks == P)
        step = 4
        first = True
        for k0 in range(0, nfull, step):
            k1 = min(nfull, k0 + step)
            src = Wap[k0 * P:k1 * P, m0:m0 + mg].rearrange("(k p) m -> p k m", p=P)
            self.dma(view[:, k0:k1, :], src, [Wbuf], wbl if first else [], [] if first else wbl, q="pool")
            first = False
        if nfull < nkc:
            k0, ks = kch[-1]
            self.dma(view[0:ks, nfull, :], Wap[k0:k0 + ks, m0:m0 + mg], [Wbuf],
                     wbl if first else [], [] if first else wbl, q="pool")
        return view, wbl

    def load_act(self, A, t0, size, kch, row0=0, dyn=False):
        at, ab = self.rot("a", self.abuf)
        nkc = len(kch)
        tw = 512 if nkc * 512 <= self.AB else 256
        assert size <= tw and nkc * tw <= self.AB
        view = at[:, 0:nkc * tw].rearrange("p (k t) -> p k t", t=tw)
        if isinstance(A, Blk):
            Aap, Abuf, b0 = A.get(t0)
        else:
            (Aap, Abuf), b0 = A, 0
        nfull = sum(1 for (_, ks) in kch if ks == P)
        step = 4
        first = True

        def mk(dst, r0, r1, full):
            def fn(e):
                tsl = slice(t0 - b0, t0 - b0 + size)
                if full:
                    src = Aap[r0:r1, tsl].rearrange("(k p) t -> p k t", p=P)
                else:
                    src = Aap[r0:r1, tsl]
                return e.dma_start(out=dst, in_=src)
            return fn

        for k0 in range(0, nfull, step):
            k1 = min(nfull, k0 + step)
            self.A("sp", mk(view[:, k0:k1, 0:size], row0 + k0 * P, row0 + k1 * P, True), [Abuf],
                   [ab] if first else [], [] if first else [ab], dma=True)
            first = False
        if nfull < nkc:
            k0, ks = kch[-1]
            self.A("sp", mk(view[0:ks, nfull, 0:size], row0 + k0, row0 + k0 + ks, False), [Abuf],
                   [ab] if first else [], [] if first else [ab], dma=True)
        return view, ab

    def gemm(self, W, K, M, act_loader, tblocks, epilogue, post_block=None, per_block=None, mg_max=1024, combined=False):
        kch = kchunks(K)
        nkc = len(kch)
        mg_cap = min(mg_max, ((2 * self.WB if combined else self.WB) // nkc) // P * P)
        groups = []
        m = 0
        while m < M:
            groups.append((m, min(mg_cap, M - m)))
            m += mg_cap
        iters = [(gi, bi) for gi in range(len(groups)) for bi in range(len(tblocks))]
        pending = act_loader(tblocks[0][0], tblocks[0][1], kch)
        for it, (gi, bi) in enumerate(iters):
            g0, mg = groups[gi]
            blk = tblocks[bi]
            if bi == 0:
                wv, wbl = self.load_weights(W, g0, mg, kch, combined=combined)
            if True:
                t0, size = blk[0], blk[1]
                av, ab = pending
                if per_block is not None and gi == 0:
                    per_block(av, ab, bi, blk)
                j = 0
                m0 = 0
                while m0 < mg:
                    msz = min(P, mg - m0)
                    pt, pb = self.psum()
                    for ki, (k0, ks) in enumerate(kch):
                        self.A("pe",
                               lambda e, pt=pt, wv=wv, av=av, ki=ki, ks=ks, m0=m0, msz=msz, size=size:
                               e.matmul(pt[0:msz, 0:size], wv[0:ks, ki, m0:m0 + msz], av[0:ks, ki, 0:size],
                                        start=(ki == 0), stop=(ki == nkc - 1)),
                               wbl + [ab], [pb] if ki == 0 else [], [] if ki == 0 else [pb])
                    epilogue(pt[0:msz, 0:size], pb, gi, j, g0 + m0, msz, bi, blk)
                    m0 += msz
                    j += 1
                if it + 1 < len(iters):
                    nblk = tblocks[iters[it + 1][1]]
                    pending = act_loader(nblk[0], nblk[1], kch)
                if post_block is not None:
                    post_block(gi, g0, mg, bi, blk)

    def std_epilogue(self, dst, func=AF.Copy, out_dt=BF16, row0=0, toff=0, eng="act"):
        state = {}
        dap, dbuf = dst

        def epi(ps, pb, gi, j, m0, msz, bi, blk):
            size = blk[1]
            if j == 0:
                state["o"] = self.rot("oh", self.obuf16) if out_dt == BF16 else self.rot("o", self.obuf)
            ot, ob = state["o"]
            ov = ot[:, :].rearrange("p (j t) -> p j t", t=512)
            if eng == "act":
                self.A("act", lambda e: e.activation(out=ov[0:msz, j, 0:size], in_=ps, func=func),
                       [pb], [ob] if j == 0 else [], [] if j == 0 else [ob])
            else:
                self.A("dve", lambda e: e.tensor_copy(out=ov[0:msz, j, 0:size], in_=ps),
                       [pb], [ob] if j == 0 else [], [] if j == 0 else [ob])

        def post(gi, g0, mg, bi, blk):
            t0, size = blk[0], blk[1]
            ot, ob = state["o"]
            ov = ot[:, :].rearrange("p (j t) -> p j t", t=512)
            nj = mg // P
            if nj > 0:
                d = dap[row0 + g0:row0 + g0 + nj * P, toff + t0:toff + t0 + size].rearrange("(j p) t -> p j t", p=P)
                self.dma(d, ov[:, 0:nj, 0:size], [ob], (), [dbuf])
            rem = mg - nj * P
            if rem:
                d = dap[row0 + g0 + nj * P:row0 + g0 + mg, toff + t0:toff + t0 + size]
                self.dma(d, ov[0:rem, nj, 0:size], [ob], (), [dbuf])

        return epi, post

    def collective(self, kind, src, dst, groups=None):
        sap, sbuf_ = src
        dap, dbuf = dst
        rg = groups or RGROUPS
        op = ALU.bypass if kind == "AllGather" else ALU.add
        if FAKECC:
            rows = sap.shape[0]
            if kind == "AllGather":
                for r in range(G):
                    self.dma(dap[r * rows:(r + 1) * rows, :], sap, [sbuf_], [dbuf] if r == 0 else [], [] if r == 0 else [dbuf])
            elif kind == "AllReduce":
                self.dma(dap, sap, [sbuf_], [dbuf])
            else:
                self.dma(dap, sap[0:dap.shape[0], :], [sbuf_], [dbuf])
            return
        self.A("pool", lambda e: e.collective_compute(kind, op, replica_groups=rg, ins=[sap], outs=[dap]),
               [sbuf_], [dbuf], cc=True)

    def build(self):
        cfg = self.cfg
        nc = self.nc
        L, D, T, TB, CH, NCH, KD = cfg.DEPTH, cfg.D, cfg.T, cfg.TB, cfg.CH, cfg.NCH, cfg.KD
        A = self.A
        I = {}
        I["xT"] = self.inp("xT", [CH, T])
        I["cT"] = self.inp("cT", [D, NR])
        I["vecs"] = self.inp("vecs", [L * P, self.NV])
        I["mod_w"] = self.inp("mod_w", [L * D, 6 * CH])
        I["w_gate"] = self.inp("w_gate", [L * D, 4 * CH])
        I["w_qkv"] = self.inp("w_qkv", [L * D, cfg.NQKV])
        I["w_v"] = self.inp("w_v", [L * D, 3 * P])
        I["w_uq"] = self.inp("w_uq", [L * 512, 256])
        I["w_ukv"] = self.inp("w_ukv", [L * 256, 256])
        I["w_br"] = self.inp("w_br", [L * 2048, CH])
        I["w_out"] = self.inp("w_out", [L * D, CH])
        I["f_w13"] = self.inp("f_w13", [cfg.ND * D, 2 * cfg.FD])
        I["f_w2"] = self.inp("f_w2", [cfg.ND * cfg.FD, D])
        if cfg.NM:
            I["m_w13"] = self.inp("m_w13", [cfg.NM * 2 * D, 2 * cfg.FE])
            I["m_w2"] = self.inp("m_w2", [cfg.NM * 2 * cfg.FE, D])
            I["router"] = self.inp("router", [cfg.NM * CH, 8])
        I["rope_a"] = self.inp("rope_a", [4 * P, TB])
        I["swa_m"] = self.inp("swa_m", [self.swa_mask.shape[0] * P, 512])
        I["na_m"] = self.inp("na_m", [self.na_mask.shape[0] * P, 512])
        I["na_b"] = self.inp("na_b", [L * self.na_mask.shape[0] * P, 512])
        self.I = I
        Dm = {}
        Dm["hT"] = self.dram("hT", [CH, T], F32)
        Dm["ss_in"] = self.dram("ss_in", [1, T], F32)
        Dm["ss_out"] = self.dram("ss_out", [1, T], F32)
        lbk = cfg.lblocks()
        Dm["xn_s"] = Blk(self, "xn_s", CH, BF16, lbk)
        Dm["xn_f"] = Blk(self, "xn_f", D, BF16, lbk)
        Dm["sg"] = self.dram("sg", [4 * CH, T], BF16)
        Dm["u"] = self.dram("u", [cfg.NQKV, TB], BF16)
        Dm["vtm"] = self.dram("vtm", [TB, 4 * P], BF16)
        Dm["qk"] = self.dram("qk", [12 * P, TB], BF16)
        Dm["o_s"] = Blk(self, "o_s", 4 * P, BF16, lbk)
        Dm["o_f"] = Blk(self, "o_f", G * 4 * P, BF16, lbk)
        Dm["mg_s"] = Blk(self, "mg_s", CH, BF16, lbk)
        Dm["mg_f"] = Blk(self, "mg_f", D, BF16, lbk)
        Dm["hid"] = self.dram("hid", [max(cfg.FD, cfg.FE if cfg.NM else 0), T], BF16)
        Dm["y_in"] = Blk(self, "y_in", D, F32, lbk)
        Dm["y_out"] = Blk(self, "y_out", CH, F32, lbk)
        if cfg.NM:
            Dm["lg_in"] = self.dram("lg_in", [T, 8], F32)
            Dm["lg_out"] = self.dram("lg_out", [T, 8], F32)
            Dm["gvec"] = self.dram("gvec", [2, T], F32)
        self.out = self.nc.dram_tensor("outT", [CH, T], F32, kind="ExternalOutput").ap()
        self.outb = Buf("outT")
        self.Dm = Dm
        self.ones16 = self.sb("ones16", [P, P], BF16)
        self.vec = self.sb("vec", [P, L, self.NV], F32)
        self.modv = self.sb("modv", [P, L * 6 * NCH * NR], F32)
        self.modA = self.sb("modA", [P, L * 4 * NCH * NR], F32)
        self.csil = self.sb("csil", [P, KD * 4], BF16)
        self.c32 = self.sb("c32", [P, KD * 4], F32)
        self.ssrow = [self.sb(f"ssrow{i}", [1, 512], F32) for i in range(2)]
        self.wv_sb = self.sb("wv_sb", [P, KD * 3 * P], BF16)
        self.wuq_sb = self.sb("wuq_sb", [P, 4 * 256], BF16)
        self.wukv_sb = self.sb("wukv_sb", [P, 2 * 256], BF16)
        self.tmpA = self.sb("tmpA", [P, 4 * 512], F32)
        self.tmpB = self.sb("tmpB", [P, 4 * 512], F32)
        self.tmpC = self.sb("tmpC", [P, 4 * 512], F32)
        self.tmpD = self.sb("tmpD", [P, 2 * 512], F32)
        self.h16 = self.sb("h16", [P, 4 * 512], BF16)
        self.h16b = self.sb("h16b", [P, 4 * 512], BF16)
        self.lamv = self.sb("lamv", [P, 16], F32)
        self.rt_sb = self.sb("rt_sb", [P, NCH * 8], F32)

        ot, ob = self.ones16
        A("dve", lambda e: e.memset(ot[:, :], 1.0), (), [ob])
        vt, vb = self.vec
        self.dma(vt[:, :, :], I["vecs"][0].rearrange("(l p) n -> p l n", p=P), [I["vecs"][1]], [vb])
        self.dma(Dm["hT"][0][:, :], I["xT"][0][:, :], [I["xT"][1]], [Dm["hT"][1]])

        try:
            self.phase(1)
            self.emit_mod()
            for l in range(L):
                self.layer(l)
            self.phase(12)
            self.norm_phase(L - 1, which="final")
        except _Stop:
            pass
        A("sp", lambda e: e.nop(), list(ALL_BUFS))
        self.S.emit()
        return self.nc

    def phase(self, k):
        if k > self.stop:
            raise _Stop()

    def vcol(self, l, name, j=0):
        vt, vb = self.vec
        c = self.vcols[name] + j
        return vt[:, l, c:c + 1]

    def mod(self, l, part, j, r):
        mt, mb = self.modv
        idx = ((l * 6 + part) * self.cfg.NCH + j) * NR + r
        return mt[:, idx:idx + 1]

    def modA_(self, l, which, j, r):
        mt, mb = self.modA
        idx = ((l * 4 + which) * self.cfg.NCH + j) * NR + r
        return mt[:, idx:idx + 1]

    def emit_mod(self):
        cfg = self.cfg
        A = self.A
        KD, NCH, L, D, CH = cfg.KD, cfg.NCH, cfg.DEPTH, cfg.D, cfg.CH
        ct, cb = self.c32
        st_, sbf = self.csil
        cv = ct[:, 0:KD * NR].rearrange("p (k r) -> p k r", r=NR)
        sv = st_[:, 0:KD * NR].rearrange("p (k r) -> p k r", r=NR)
        self.dma(cv, self.I["cT"][0].rearrange("(k p) r -> p k r", p=P), [self.I["cT"][1]], [cb])
        A("act", lambda e: e.activation(out=sv, in_=cv, func=AF.Silu), [cb], [sbf])
        mt, mb = self.modv
        vt, vb = self.vec
        first = [True]
        for l in range(L):
            W = (self.I["mod_w"][0][l * D:(l + 1) * D, :], self.I["mod_w"][1])

            def loader(t0, size, kch):
                return sv, sbf

            def epi(ps, pb, gi, j, m0, msz, bi, blk, l=l):
                jj = m0 // P
                idx = (l * 6 * NCH + jj) * NR
                bcol = self.vcols["modb"] + jj
                A("dve", lambda e: e.tensor_scalar(out=mt[:, idx:idx + NR], in0=ps, scalar1=vt[:, l, bcol:bcol + 1],
                                                   scalar2=None, op0=ALU.add),
                  [pb, vb], [mb] if first[0] else [], [] if first[0] else [mb])
                first[0] = False

            self.gemm(W, D, 6 * CH, loader, [(0, NR)], epi)
        at, ab = self.modA
        firstA = True
        for l in range(L):
            for which, (gname, part) in enumerate((("n1g", 1), ("n2g", 4))):
                for j in range(NCH):
                    src = ((l * 6 + part) * NCH + j) * NR
                    dst = ((l * 4 + which) * NCH + j) * NR
                    g = self.vcol(l, gname, j)
                    A("dve", lambda e, src=src, dst=dst, g=g: e.tensor_scalar(
                        out=at[:, dst:dst + NR], in0=mt[:, src:src + NR], scalar1=1.0, scalar2=g,
                        op0=ALU.add, op1=ALU.mult),
                      [mb, vb], [ab] if firstA else [], [] if firstA else [ab])
                    firstA = False

    def norm_phase(self, l, which):
        cfg = self.cfg
        A = self.A
        NCH, T, D = cfg.NCH, cfg.T, cfg.D
        Dm = self.Dm
        hT, hTb = Dm["hT"]
        ones, onesb = self.ones16
        blocks = cfg.gblocks()
        moe = (which == 2 and l % 2 == 1)
        first = True
        for (t0, size, r) in blocks:
            ht, hb = self.tmpA
            hv = ht[:, 0:NCH * 512].rearrange("p (j t) -> p j t", t=512)
            self.dma(hv[:, :, 0:size], hT[:, t0:t0 + size].rearrange("(j p) t -> p j t", p=P), [hTb], [hb])
            qt, qb = self.h16
            qv = qt[:, 0:NCH * 512].rearrange("p (j t) -> p j t", t=512)
            A("act", lambda e, qv=qv, hv=hv, size=size: e.activation(out=qv[:, :, 0:size], in_=hv[:, :, 0:size], func=AF.Square),
              [hb], [qb])
            pt, pb = self.psum()
            for j in range(NCH):
                A("pe", lambda e, pt=pt, qv=qv, j=j, size=size: e.matmul(pt[0:1, 0:size], ones[:, 0:1], qv[:, j, 0:size],
                                                                         start=(j == 0), stop=(j == NCH - 1)),
                  [onesb, qb], [pb] if j == 0 else [], [] if j == 0 else [pb])
            srow, srowb = self.ssrow[self.cnt["o"] % 2]
            self.cnt["o"] += 1
            A("dve", lambda e, pt=pt, srow=srow, size=size: e.tensor_copy(out=srow[0:1, 0:size], in_=pt[0:1, 0:size]),
              [pb], [srowb])
            self.dma(Dm["ss_in"][0][0:1, t0:t0 + size], srow[0:1, 0:size], [srowb], [Dm["ss_in"][1]] if first else [], [] if first else [Dm["ss_in"][1]])
            first = False
        self.collective("AllReduce", Dm["ss_in"], Dm["ss_out"])
        for (t0, size, r) in blocks:
            bt, bb = self.tmpB
            self.dma(bt[:, 0:size], Dm["ss_out"][0][0:1, t0:t0 + size].partition_broadcast(P), [Dm["ss_out"][1]], [bb])
            A("act", lambda e, bt=bt, size=size: e.activation(out=bt[:, 0:size], in_=bt[:, 0:size], func=AF.Sqrt,
                                                              scale=1.0 / D, bias=cfg.eps), [bb], [bb])
            A("dve", lambda e, bt=bt, size=size: e.reciprocal(out=bt[:, 0:size], in_=bt[:, 0:size]), [bb], [bb])
            ht, hb = self.tmpA
            hv = ht[:, 0:NCH * 512].rearrange("p (j t) -> p j t", t=512)
            self.dma(hv[:, :, 0:size], hT[:, t0:t0 + size].rearrange("(j p) t -> p j t", p=P), [hTb], [hb])
            xt, xb = self.tmpC
            xv = xt[:, 0:NCH * 512].rearrange("p (j t) -> p j t", t=512)
            for j in range(NCH):
                if which == "final":
                    gain = self.vcol(0, "fng", j)
                    shift = None
                else:
                    gain = self.modA_(l, 0 if which == 1 else 1, j, r)
                    shift = self.mod(l, 0 if which == 1 else 3, j, r)
                A("dve", lambda e, xv=xv, hv=hv, bt=bt, j=j, size=size, gain=gain: e.scalar_tensor_tensor(
                    out=xv[:, j, 0:size], in0=hv[:, j, 0:size], scalar=gain, in1=bt[:, 0:size],
                    op0=ALU.mult, op1=ALU.mult),
                  [hb, bb, self.modA[1], self.vec[1]], [xb] if j == 0 else [], [] if j == 0 else [xb])
                if shift is not None:
                    A("dve", lambda e, xv=xv, j=j, size=size, shift=shift: e.tensor_scalar(
                        out=xv[:, j, 0:size], in0=xv[:, j, 0:size], scalar1=shift, scalar2=None, op0=ALU.add),
                      [xb, self.modv[1]], (), [xb])
            if which == "final":
                self.dma(self.out[:, t0:t0 + size].rearrange("(j p) t -> p j t", p=P), xv[:, :, 0:size], [xb], (), [self.outb])
                continue
            yt, yb = self.h16b
            yv = yt[:, 0:NCH * 512].rearrange("p (j t) -> p j t", t=512)
            A("act", lambda e, yv=yv, xv=xv, size=size: e.activation(out=yv[:, :, 0:size], in_=xv[:, :, 0:size], func=AF.Copy),
              [xb], [yb])
            xs_ap, xs_buf, xb0 = Dm["xn_s"].get(t0)
            self.dma(xs_ap[:, t0 - xb0:t0 - xb0 + size].rearrange("(j p) t -> p j t", p=P), yv[:, :, 0:size], [yb], [xs_buf])
            xf_ap, xf_buf, _ = Dm["xn_f"].get(t0)
            self.collective("AllGather", (xs_ap, xs_buf), (xf_ap, xf_buf))
            if moe:
                rt, rb = self.rt_sb
                rv = rt[:, :].rearrange("p (j e) -> p j e", e=8)
                if t0 == 0:
                    ri = l // 2
                    self.dma(rv, self.I["router"][0][ri * cfg.CH:(ri + 1) * cfg.CH, :].rearrange("(j p) e -> p j e", p=P), [self.I["router"][1]], [rb])
                for tt in range(size // P):
                    pt, pb = self.psum()
                    for j in range(NCH):
                        A("pe", lambda e, pt=pt, xv=xv, rv=rv, j=j, tt=tt: e.matmul(
                            pt[:, 0:8], xv[:, j, tt * P:(tt + 1) * P], rv[:, j, :], start=(j == 0), stop=(j == NCH - 1)),
                          [xb, rb], [pb] if j == 0 else [], [] if j == 0 else [pb])
                    lt, lb = self.tmpD
                    A("dve", lambda e, pt=pt, lt=lt: e.tensor_copy(out=lt[:, 0:8], in_=pt[:, 0:8]), [pb], [lb])
                    self.dma(Dm["lg_in"][0][t0 + tt * P:t0 + (tt + 1) * P, :], lt[:, 0:8], [lb], (), [Dm["lg_in"][1]])

    def layer(self, l):
        cfg = self.cfg
        A = self.A
        D, T, TB, CH, NCH, KD = cfg.D, cfg.T, cfg.TB, cfg.CH, cfg.NCH, cfg.KD
        I, Dm = self.I, self.Dm
        lb = cfg.lblocks()
        gb = cfg.gblocks()
        self.phase(2)
        self.norm_phase(l, 1)
        self.phase(3)
        wvt, wvb = self.wv_sb
        wvv = wvt[:, :].rearrange("p (k m) -> p k m", m=3 * P)
        self.load_weights((I["w_v"][0][l * D:(l + 1) * D, :], I["w_v"][1]), 0, 3 * P, kchunks(D), dst=(wvv, wvb))
        uqt, uqb = self.wuq_sb
        uqv = uqt[:, :].rearrange("p (k m) -> p k m", m=256)
        self.load_weights((I["w_uq"][0][l * 512:(l + 1) * 512, :], I["w_uq"][1]), 0, 256, kchunks(512), dst=(uqv, uqb))
        ukt, ukb = self.wukv_sb
        ukv = ukt[:, :].rearrange("p (k m) -> p k m", m=256)
        self.load_weights((I["w_ukv"][0][l * 256:(l + 1) * 256, :], I["w_ukv"][1]), 0, 256, kchunks(256), dst=(ukv, ukb))
        epi, post = self.std_epilogue(Dm["sg"], func=AF.Sigmoid)
        self.gemm((I["w_gate"][0][l * D:(l + 1) * D, :], I["w_gate"][1]), D, 4 * CH,
                  lambda t0, size, kch: self.load_act(Dm["xn_f"], t0, size, kch), gb, epi, post)
        self.phase(4)
        epi, post = self.std_epilogue(Dm["u"])

        def vhook(av, ab, bi, blk):
            t0, size = blk[0], blk[1]
            for tt in range(size // P):
                pt, pb = self.psum()
                for k in range(KD):
                    A("pe", lambda e, pt=pt, av=av, k=k, tt=tt: e.matmul(
                        pt[:, 0:3 * P], av[:, k, tt * P:(tt + 1) * P], wvv[:, k, :], start=(k == 0), stop=(k == KD - 1)),
                      [ab, wvb], [pb] if k == 0 else [], [] if k == 0 else [pb])
                vt_, vb_ = self.h16
                A("act", lambda e, pt=pt, vt_=vt_: e.activation(out=vt_[:, 0:3 * P], in_=pt[:, 0:3 * P], func=AF.Copy), [pb], [vb_])
                self.dma(Dm["vtm"][0][t0 + tt * P:t0 + (tt + 1) * P, 0:3 * P], vt_[:, 0:3 * P], [vb_], (), [Dm["vtm"][1]])

        self.gemm((I["w_qkv"][0][l * D:(l + 1) * D, :], I["w_qkv"][1]), D, cfg.NQKV,
                  lambda t0, size, kch: self.load_act(Dm["xn_f"], t0, size, kch), lb, epi, post,
                  per_block=vhook)
        self.phase(5)
        self.mla_prep(l)
        self.phase(6)
        self.rope_phase(l)
        self.phase(7)
        self.attn_all(l)
        self.phase(8)
        for (t0_, sz_, ap_s, buf_s), (_, _, ap_f, buf_f) in zip(Dm["o_s"].items, Dm["o_f"].items):
            self.collective("AllGather", (ap_s, buf_s), (ap_f, buf_f))
        self.merge_phase(l)
        self.phase(9)
        self.resid_gemm(l)
        self.phase(10)
        self.norm_phase(l, 2)
        self.phase(11)
        self.ffn_phase(l)

    def colnorm(self, xv, xb, nch, size, nfeat, gains, outv, outb):
        A = self.A
        ones, onesb = self.ones16
        qt, qb = self.h16b
        qv = qt[:, 0:nch * 512].rearrange("p (j t) -> p j t", t=512)
        A("act", lambda e: e.activation(out=qv[:, 0:nch, 0:size], in_=xv[:, 0:nch, 0:size], func=AF.Square), [xb], [qb])
        pt, pb = self.psum()
        for j in range(nch):
            A("pe", lambda e, j=j: e.matmul(pt[:, 0:size], ones[:, :], qv[:, j, 0:size], start=(j == 0), stop=(j == nch - 1)),
              [onesb, qb], [pb] if j == 0 else [], [] if j == 0 else [pb])
        rt, rb = self.tmpD
        A("act", lambda e: e.activation(out=rt[:, 0:size], in_=pt[:, 0:size], func=AF.Sqrt, scale=1.0 / nfeat, bias=self.cfg.eps),
          [pb], [rb])
        A("dve", lambda e: e.reciprocal(out=rt[:, 0:size], in_=rt[:, 0:size]), [rb], [rb])
        for j in range(nch):
            A("dve", lambda e, j=j: e.scalar_tensor_tensor(out=outv[:, j, 0:size], in0=xv[:, j, 0:size], scalar=gains[j],
                                                           in1=rt[:, 0:size], op0=ALU.mult, op1=ALU.mult),
              [xb, rb, self.vec[1]], [outb] if j == 0 else [], [] if j == 0 else [outb])

    def mla_prep(self, l):
        cfg = self.cfg
        A = self.A
        Dm = self.Dm
        u, ub = Dm["u"]
        qk, qkb = Dm["qk"]
        uqt, uqb = self.wuq_sb
        uqv = uqt[:, :].rearrange("p (k m) -> p k m", m=256)
        ukt, ukb = self.wukv_sb
        ukv = ukt[:, :].rearrange("p (k m) -> p k m", m=256)
        for (t0, size, kind) in cfg.lblocks():
            self._mla_block(l, t0, size)
        u_ap = u
        self.dma(qk[8 * P + 64:9 * P, :], u_ap[768:832, :], [ub], (), [qkb])
        self.dma(qk[9 * P + 64:10 * P, :], u_ap[832:896, :], [ub], (), [qkb])

    def _mla_block(self, l, t0, size):
        cfg = self.cfg
        A = self.A
        Dm = self.Dm
        u, ub = Dm["u"]
        qk, qkb = Dm["qk"]
        uqt, uqb = self.wuq_sb
        uqv = uqt[:, :].rearrange("p (k m) -> p k m", m=256)
        ukt, ukb = self.wukv_sb
        ukv = ukt[:, :].rearrange("p (k m) -> p k m", m=256)
        if True:
            av, ab = self.load_act(Dm["u"], t0, size, kchunks(512), row0=0)
            nt, nb = self.h16
            nv = nt[:, 0:4 * 512].rearrange("p (j t) -> p j t", t=512)
            self.colnorm(av, ab, 4, size, 512, [self.vcol(l, "qng", j) for j in range(4)], nv, nb)
            ot, ob = self.rot("oh", self.obuf16)
            ov = ot[:, :].rearrange("p (j t) -> p j t", t=512)
            for mc in range(2):
                pt, pb = self.psum()
                for k in range(4):
                    A("pe", lambda e, pt=pt, k=k, mc=mc: e.matmul(pt[:, 0:size], uqv[:, k, mc * P:(mc + 1) * P], nv[:, k, 0:size],
                                                                 start=(k == 0), stop=(k == 3)),
                      [uqb, nb], [pb] if k == 0 else [], [] if k == 0 else [pb])
                A("act", lambda e, pt=pt, mc=mc: e.activation(out=ov[:, mc, 0:size], in_=pt[:, 0:size], func=AF.Copy),
                  [pb], [ob] if mc == 0 else [], [] if mc == 0 else [ob])
            self.dma(qk[0:P, t0:t0 + size], ov[:, 0, 0:size], [ob], (), [qkb])
            self.dma(qk[8 * P:8 * P + 64, t0:t0 + size], ov[0:64, 1, 0:size], [ob], (), [qkb])
            self.dma(qk[9 * P:9 * P + 64, t0:t0 + size], ov[64:128, 1, 0:size], [ob], (), [qkb])
            self._mla_block2(l, t0, size)

    def _mla_block2(self, l, t0, size):
        cfg = self.cfg
        A = self.A
        Dm = self.Dm
        qk, qkb = Dm["qk"]
        ukt, ukb = self.wukv_sb
        ukv = ukt[:, :].rearrange("p (k m) -> p k m", m=256)
        if True:
            av, ab = self.load_act(Dm["u"], t0, size, kchunks(256), row0=512)
            nt, nb = self.h16
            nv = nt[:, 0:4 * 512].rearrange("p (j t) -> p j t", t=512)
            self.colnorm(av, ab, 2, size, 256, [self.vcol(l, "kvng", j) for j in range(2)], nv, nb)
            ot, ob = self.rot("oh", self.obuf16)
            ov = ot[:, :].rearrange("p (j t) -> p j t", t=512)
            pt, pb = self.psum()
            for k in range(2):
                A("pe", lambda e, pt=pt, k=k: e.matmul(pt[:, 0:size], ukv[:, k, 0:P], nv[:, k, 0:size], start=(k == 0), stop=(k == 1)),
                  [ukb, nb], [pb] if k == 0 else [], [] if k == 0 else [pb])
            A("act", lambda e, pt=pt: e.activation(out=ov[:, 0, 0:size], in_=pt[:, 0:size], func=AF.Copy), [pb], [ob])
            self.dma(qk[2 * P:3 * P, t0:t0 + size], ov[:, 0, 0:size], [ob], (), [qkb])
            for tt in range(size // P):
                pt, pb = self.psum()
                for k in range(2):
                    A("pe", lambda e, pt=pt, k=k, tt=tt: e.matmul(pt[:, 0:P], nv[:, k, tt * P:(tt + 1) * P], ukv[:, k, P:2 * P],
                                                                 start=(k == 0), stop=(k == 1)),
                      [ukb, nb], [pb] if k == 0 else [], [] if k == 0 else [pb])
                A("act", lambda e, pt=pt, tt=tt: e.activation(out=ov[:, 1 + tt // 4, (tt % 4) * P:(tt % 4 + 1) * P], in_=pt[:, 0:P], func=AF.Copy),
                  [pb], (), [ob])
                self.dma(Dm["vtm"][0][t0 + tt * P:t0 + (tt + 1) * P, 3 * P:4 * P], ov[:, 1 + tt // 4, (tt % 4) * P:(tt % 4 + 1) * P],
                         [ob], (), [Dm["vtm"][1]])

    def rope_phase(self, l):
        cfg = self.cfg
        A = self.A
        Dm = self.Dm
        u, ub = Dm["u"]
        qk, qkb = Dm["qk"]
        ra, rab = self.I["rope_a"]
        jobs = [
            (qk, qkb, 8 * P, 9 * P, 0, [(0, 64, 1 * P), (64, 128, 3 * P)]),
            (u, ub, 7 * P, 8 * P, 1, [(0, 128, 4 * P)]),
            (u, ub, 9 * P, 10 * P, 1, [(0, 128, 5 * P)]),
            (u, ub, 13 * P, 14 * P, 0, [(0, 128, 10 * P)]),
            (u, ub, 15 * P, 16 * P, 0, [(0, 128, 11 * P)]),
        ]
        for (src, srcb, rx, rp, tab, outs) in jobs:
            for (t0, size, kind) in cfg.lblocks():
                xt, xb = self.h16
                pt_, pb_ = self.h16b
                self.dma(xt[:, 0:size], src[rx:rx + P, t0:t0 + size], [srcb], [xb])
                self.dma(pt_[:, 0:size], src[rp:rp + P, t0:t0 + size], [srcb], [pb_])
                ct, cb = self.tmpA
                st_, sb_ = self.tmpB
                self.dma(ct[:, 0:size], ra[(2 * tab) * P:(2 * tab + 1) * P, t0:t0 + size], [rab], [cb])
                self.dma(st_[:, 0:size], ra[(2 * tab + 1) * P:(2 * tab + 2) * P, t0:t0 + size], [rab], [sb_])
                A("dve", lambda e, ct=ct, xt=xt, size=size: e.tensor_tensor(out=ct[:, 0:size], in0=xt[:, 0:size], in1=ct[:, 0:size], op=ALU.mult),
                  [xb, cb], [cb])
                A("dve", lambda e, st_=st_, pt_=pt_, size=size: e.tensor_tensor(out=st_[:, 0:size], in0=pt_[:, 0:size], in1=st_[:, 0:size], op=ALU.mult),
                  [pb_, sb_], [sb_])
                ot, ob = self.rot("oh", self.obuf16)
                A("dve", lambda e, ot=ot, ct=ct, st_=st_, size=size: e.tensor_tensor(out=ot[:, 0:size], in0=ct[:, 0:size], in1=st_[:, 0:size], op=ALU.add),
                  [cb, sb_], [ob])
                for (p0, p1, d0) in outs:
                    self.dma(qk[d0:d0 + (p1 - p0), t0:t0 + size], ot[p0:p1, 0:size], [ob], (), [qkb])
        self.dma(qk[6 * P:7 * P, :], u[11 * P:12 * P, :], [ub], (), [qkb])
        self.dma(qk[7 * P:8 * P, :], u[12 * P:13 * P, :], [ub], (), [qkb])

    def attn_all(self, l):
        cfg = self.cfg
        A = self.A
        Dm = self.Dm
        nct = cfg.CTX // P
        nkt = cfg.TB // P
        nq = cfg.SEQ // 512
        dense = [[(kt, None) for kt in range(nkt)] for _ in range(nq)]
        ctxs = [(kt, None) for kt in range(nct)]
        et, eb = self.abuf[0]
        nsw = self.swa_mask.shape[0]
        nna = self.na_mask.shape[0]
        assert (nsw + nna) * 512 <= self.AB
        ev = et[:, 0:(nsw + nna) * 512].rearrange("p (n t) -> p n t", t=512)
        first = True
        for i in range(nsw):
            tt, tb = self.tmpA
            self.dma(tt[:, 0:512], self.I["swa_m"][0][i * P:(i + 1) * P, :], [self.I["swa_m"][1]], [tb])
            A("act", lambda e, i=i, tt=tt: e.activation(out=ev[:, i, :], in_=tt[:, 0:512], func=AF.Copy), [tb],
              [eb] if first else [], [] if first else [eb])
            first = False
        for i in range(nna):
            tt, tb = self.tmpA
            mt, mb = self.tmpB
            self.dma(tt[:, 0:512], self.I["na_b"][0][(l * nna + i) * P:(l * nna + i + 1) * P, :], [self.I["na_b"][1]], [tb])
            self.dma(mt[:, 0:512], self.I["na_m"][0][i * P:(i + 1) * P, :], [self.I["na_m"][1]], [mb])
            A("act", lambda e, tt=tt: e.activation(out=tt[:, 0:512], in_=tt[:, 0:512], func=AF.Exp), [tb], [tb])
            A("dve", lambda e, i=i, tt=tt, mt=mt: e.tensor_tensor(out=ev[:, nsw + i, :], in0=tt[:, 0:512], in1=mt[:, 0:512], op=ALU.mult),
              [tb, mb], (), [eb])
        lt, lb = self.lamv
        A("act", lambda e: e.activation(out=lt[:, 0:1], in_=self.vcol(l, "sink"), func=AF.Exp), [self.vec[1]], [lb])
        lam_init = 0.8 - 0.6 * math.exp(-0.3 * l)
        vt, vb = self.vec
        c0 = self.vcols["lam4"]
        pr, prb = self.tmpD
        for a in range(2):
            A("dve", lambda e, a=a: e.tensor_tensor(out=pr[:, a * 64:(a + 1) * 64], in0=vt[:, l, c0 + (2 * a) * 64:c0 + (2 * a + 1) * 64],
                                                    in1=vt[:, l, c0 + (2 * a + 1) * 64:c0 + (2 * a + 2) * 64], op=ALU.mult),
              [vb], [prb] if a == 0 else [], [] if a == 0 else [prb])
            A("dve", lambda e, a=a: e.reduce_sum(out=lt[:, 2 + a:3 + a], in_=pr[:, a * 64:(a + 1) * 64], axis=AX.X), [prb], (), [lb])
        A("act", lambda e: e.activation(out=lt[:, 2:4], in_=lt[:, 2:4], func=AF.Exp), [lb], (), [lb])
        A("dve", lambda e: e.tensor_tensor(out=lt[:, 4:5], in0=lt[:, 3:4], in1=lt[:, 2:3], op=ALU.subtract), [lb], (), [lb])
        A("dve", lambda e: e.tensor_scalar(out=lt[:, 4:5], in0=lt[:, 4:5], scalar1=-lam_init, scalar2=None, op0=ALU.add), [lb], (), [lb])
        A("dve", lambda e: e.tensor_scalar(out=lt[:, 5:6], in0=self.vcol(l, "subg"), scalar1=(1.0 - lam_init), scalar2=None, op0=ALU.mult),
          [vb, lb], (), [lb])

        def sched_for(kind):
            qb = []
            if kind == "swa":
                for Q in range(nq):
                    qb.append((cfg.CTX + Q * 512, 512, ctxs + [(nct + kt, None if m is None else m) for (kt, m) in self.swa_s[Q]]))
            elif kind == "na":
                for Q in range(nq):
                    qb.append((cfg.CTX + Q * 512, 512, ctxs + [(nct + kt, nsw + m) for (kt, m) in self.na_s[Q]]))
            else:
                for Q in range(nq):
                    qb.append((cfg.CTX + Q * 512, 512, dense[Q]))
            t = 0
            while t < cfg.CTX:
                s = min(512, cfg.CTX - t)
                qb.append((t, s, ctxs))
                t += s
            return qb

        mixers = [
            ("mla", [(0, 128), (1 * P, 64)], [(2 * P, 128), (3 * P, 64)], 3, 192 ** -0.5, 0),
            ("swa", [(4 * P, 128)], [(5 * P, 128)], 0, 128 ** -0.5, 1),
            ("na", [(6 * P, 128)], [(7 * P, 128)], 1, 128 ** -0.5, 2),
            ("diff", [(10 * P, 128)], [(11 * P, 128)], 2, 64 ** -0.5, 3),
        ]
        p1t, ab1 = self.abuf[1]
        A("dve", lambda e: e.memset(p1t[:, 0:8], 0.0), (), [ab1] + list(self.pslots))
        for (name, qrows, krows, vcol, scale, om) in mixers:
            self.attention(l, name, qrows, krows, vcol, scale, om, sched_for(name), ev, eb)
        A("dve", lambda e: e.memset(p1t[:, 0:8], 0.0), (), [ab1] + list(self.pslots))

    def attention(self, l, name, qrows, krows, vcol, scale, om, qblocks, ev, eb):
        cfg = self.cfg
        A = self.A
        Dm = self.Dm
        TB = cfg.TB
        nkt = TB // P
        qk, qkb = Dm["qk"]
        ones, onesb = self.ones16
        lt, lb = self.lamv
        kt_, kb = self.wbuf[0]
        nck = len(krows)
        assert nck * TB <= self.WB and nkt * P <= self.WB
        kv = kt_[:, 0:nck * TB].rearrange("p (c t) -> p c t", t=TB)
        for ci, (r0, nr) in enumerate(krows):
            self.dma(kv[0:nr, ci, :], qk[r0:r0 + nr, :], [qkb], [kb] if ci == 0 else [], [] if ci == 0 else [kb])
        vt_, vb_ = self.wbuf[1]
        vv = vt_[:, 0:nkt * P].rearrange("p (n d) -> p n d", d=P)
        self.dma(vv, Dm["vtm"][0][:, vcol * P:(vcol + 1) * P].rearrange("(n p) d -> p n d", p=P), [Dm["vtm"][1]], [vb_])
        diff = (name == "diff")
        nmaps = 2 if diff else 1
        ACC = (0, 1, 2, 3)
        SP_ = (4, 5, 6, 7)
        for (q0, qs, klist) in qblocks:
            self._attn_qblock(name, qrows, krows, scale, om, q0, qs, klist, ev, eb, kv, kb, vv, vb_, nck, diff, nmaps)

    def _attn_qblock(self, name, qrows, krows, scale, om, q0, qs, klist, ev, eb, kv, kb, vv, vb_, nck, diff, nmaps):
        cfg = self.cfg
        A = self.A
        Dm = self.Dm
        qk, qkb = Dm["qk"]
        ones, onesb = self.ones16
        lt, lb = self.lamv
        ACC = (0, 1, 2, 3)
        SP_ = (4, 5, 6, 7)
        if True:
            qt_, qb_ = self.rot("oh", self.obuf16)
            qv = qt_[:, :].rearrange("p (j t) -> p j t", t=512)
            for ci, (r0, nr) in enumerate(qrows):
                self.dma(qv[0:nr, ci, 0:qs], qk[r0:r0 + nr, q0:q0 + qs], [qkb], [qb_] if ci == 0 else [], [] if ci == 0 else [qb_])
            accs = [(self.psum(ACC), self.psum(ACC)) for _ in range(nmaps)]
            nk = len(klist)
            pt_, ab1 = self.abuf[1]
            jobs = [(ki, kt, etile, mp) for ki, (kt, etile) in enumerate(klist) for mp in range(nmaps)]

            def emit_s(job):
                ki, kt, etile, mp = job
                ps, psb = self.psum(SP_)
                if diff:
                    A("pe", lambda e: e.matmul(ps[:, 0:qs], kv[mp * 64:(mp + 1) * 64, 0, kt * P:(kt + 1) * P],
                                               qv[mp * 64:(mp + 1) * 64, 0, 0:qs], start=True, stop=True),
                      [kb, qb_], [psb])
                else:
                    for ci, (r0, nr) in enumerate(krows):
                        A("pe", lambda e, ci=ci, nr=nr: e.matmul(ps[:, 0:qs], kv[0:nr, ci, kt * P:(kt + 1) * P],
                                                                 qv[0:nr, ci, 0:qs], start=(ci == 0), stop=(ci == nck - 1)),
                          [kb, qb_], [psb] if ci == 0 else [], [] if ci == 0 else [psb])
                return ps, psb

            def emit_rest(job, ps, psb):
                ki, kt, etile, mp = job
                (po, pob), (pd, pdb) = accs[mp]
                slot = (self.cnt["oh"] % 8)
                self.cnt["oh"] += 1
                pbuf = self.pslots[slot]
                pv = pt_[:, slot * 512:(slot + 1) * 512]
                A("act", lambda e: e.activation(out=pv[:, 0:qs], in_=ps[:, 0:qs], func=AF.Exp, scale=scale), [psb], [pbuf])
                if etile is not None:
                    A("dve", lambda e: e.tensor_tensor(out=pv[:, 0:qs], in0=pv[:, 0:qs], in1=ev[:, etile, 0:qs], op=ALU.mult),
                      [pbuf, eb], [pbuf])
                A("pe", lambda e: e.matmul(po[:, 0:qs], vv[:, kt, :], pv[:, 0:qs], start=(ki == 0), stop=(ki == nk - 1)),
                  [vb_, pbuf], [pob] if ki == 0 else [], [] if ki == 0 else [pob])
                A("pe", lambda e: e.matmul(pd[:, 0:qs], ones[:, :], pv[:, 0:qs], start=(ki == 0), stop=(ki == nk - 1)),
                  [onesb, pbuf], [pdb] if ki == 0 else [], [] if ki == 0 else [pdb])

            LA = 2
            pend = []
            for job in jobs:
                pend.append((job,) + emit_s(job))
                if len(pend) > LA:
                    emit_rest(*pend.pop(0))
            while pend:
                emit_rest(*pend.pop(0))
            res = []
            for mp in range(nmaps):
                (po, pob), (pd, pdb) = accs[mp]
                rt, rb = self.tmpA if mp == 0 else self.tmpB
                if name == "swa":
                    A("dve", lambda e, rt=rt, pd=pd: e.tensor_scalar(out=rt[:, 0:qs], in0=pd[:, 0:qs], scalar1=lt[:, 0:1], scalar2=None, op0=ALU.add),
                      [pdb, lb], [rb])
                    A("dve", lambda e, rt=rt: e.reciprocal(out=rt[:, 0:qs], in_=rt[:, 0:qs]), [rb], [rb])
                else:
                    A("dve", lambda e, rt=rt, pd=pd: e.reciprocal(out=rt[:, 0:qs], in_=pd[:, 0:qs]), [pdb], [rb])
                A("dve", lambda e, rt=rt, po=po: e.tensor_tensor(out=rt[:, 0:qs], in0=po[:, 0:qs], in1=rt[:, 0:qs], op=ALU.mult), [pob, rb], [rb])
                res.append((rt, rb))
            ot, ob = self.rot("oh", self.obuf16)
            if not diff:
                rt, rb = res[0]
                A("act", lambda e, ot=ot, rt=rt: e.activation(out=ot[:, 0:qs], in_=rt[:, 0:qs], func=AF.Copy), [rb], [ob])
            else:
                (r1, r1b), (r2, r2b) = res
                A("dve", lambda e, r1=r1, r2=r2: e.scalar_tensor_tensor(out=r1[:, 0:qs], in0=r2[:, 0:qs], scalar=lt[:, 4:5], in1=r1[:, 0:qs],
                                                                        op0=ALU.mult, op1=ALU.add), [r1b, r2b, lb], [r1b])
                qq, qqb = self.h16
                A("act", lambda e, qq=qq, r1=r1: e.activation(out=qq[:, 0:qs], in_=r1[:, 0:qs], func=AF.Square), [r1b], [qqb])
                pn, pnb = self.psum(SP_)
                A("pe", lambda e, pn=pn, qq=qq: e.matmul(pn[:, 0:qs], ones[:, :], qq[:, 0:qs], start=True, stop=True), [onesb, qqb], [pnb])
                A("act", lambda e, r2=r2, pn=pn: e.activation(out=r2[:, 0:qs], in_=pn[:, 0:qs], func=AF.Sqrt, scale=1.0 / 128, bias=cfg.eps), [pnb], [r2b])
                A("dve", lambda e, r2=r2: e.reciprocal(out=r2[:, 0:qs], in_=r2[:, 0:qs]), [r2b], [r2b])
                A("dve", lambda e, ot=ot, r1=r1, r2=r2: e.scalar_tensor_tensor(out=ot[:, 0:qs], in0=r1[:, 0:qs], scalar=lt[:, 5:6], in1=r2[:, 0:qs],
                                                                               op0=ALU.mult, op1=ALU.mult), [r1b, r2b, lb], [ob])
            os_ap, os_buf, ob0 = Dm["o_s"].get(q0)
            self.dma(os_ap[om * P:(om + 1) * P, q0 - ob0:q0 - ob0 + qs], ot[:, 0:qs], [ob], (), [os_buf])

    def merge_phase(self, l):
        cfg = self.cfg
        A = self.A
        Dm, I = self.Dm, self.I
        D, CH, NCH, TB = cfg.D, cfg.CH, cfg.NCH, cfg.TB
        wv, wbl_ = self.load_weights((I["w_br"][0][l * 2048:(l + 1) * 2048, :], I["w_br"][1]), 0, CH, kchunks(2048))
        wb = wbl_[0]
        for (t0, size, kind) in cfg.lblocks():
            self._merge_block(0, t0, size, wv, wb)

    def _merge_block(self, b, t0, size, wv, wb):
        cfg = self.cfg
        A = self.A
        Dm = self.Dm
        NCH = cfg.NCH
        av, ab = self.load_act(Dm["o_f"], t0, size, kchunks(2048), row0=0)
        ot, ob = self.h16b
        ov = ot[:, 0:NCH * 512].rearrange("p (j t) -> p j t", t=512)
        acc, accb = self.tmpA
        accv = acc[:, 0:NCH * 512].rearrange("p (j t) -> p j t", t=512)
        for mc in range(NCH):
            self._merge_mc(mc, t0, size, wv, wb, av, ab, accv, accb)
        A("act", lambda e: e.activation(out=ov[:, :, 0:size], in_=accv[:, :, 0:size], func=AF.Copy), [accb], [ob])
        ms_ap, ms_buf, mb0 = Dm["mg_s"].get(t0)
        self.dma(ms_ap[:, t0 - mb0:t0 - mb0 + size].rearrange("(j p) t -> p j t", p=P), ov[:, :, 0:size], [ob], [ms_buf])
        mf_ap, mf_buf, _ = Dm["mg_f"].get(t0)
        self.collective("AllGather", (ms_ap, ms_buf), (mf_ap, mf_buf))

    def _merge_mc(self, mc, t0, size, wv, wb, av, ab, accv, accb):
        cfg = self.cfg
        A = self.A
        Dm = self.Dm
        NCH = cfg.NCH
        gt, gb_ = self.rot("oh", self.obuf16)
        gv = gt[:, :].rearrange("p (j t) -> p j t", t=512)
        for m in range(4):
            r0 = (m * NCH + mc) * P
            self.dma(gv[:, m, 0:size], Dm["sg"][0][r0:r0 + P, t0:t0 + size], [Dm["sg"][1]], [gb_] if m == 0 else [], [] if m == 0 else [gb_])
        tmp, tmpb = self.tmpB
        for m in range(4):
            pt, pb = self.psum()
            for ih in range(4):
                k = ih * 4 + m
                A("pe", lambda e, pt=pt, k=k, ih=ih: e.matmul(pt[:, 0:size], wv[:, k, mc * P:(mc + 1) * P], av[:, k, 0:size],
                                                             start=(ih == 0), stop=(ih == 3)),
                  [wb, ab], [pb] if ih == 0 else [], [] if ih == 0 else [pb])
            if m == 0:
                A("dve", lambda e, pt=pt, m=m: e.tensor_tensor(out=accv[:, mc, 0:size], in0=pt[:, 0:size], in1=gv[:, m, 0:size], op=ALU.mult),
                  [pb, gb_], [accb] if mc == 0 else [], [] if mc == 0 else [accb])
            else:
                A("dve", lambda e, pt=pt, m=m: e.tensor_tensor(out=tmp[:, 0:size], in0=pt[:, 0:size], in1=gv[:, m, 0:size], op=ALU.mult),
                  [pb, gb_], [tmpb])
                A("dve", lambda e: e.tensor_tensor(out=accv[:, mc, 0:size], in0=accv[:, mc, 0:size], in1=tmp[:, 0:size], op=ALU.add),
                  [tmpb, accb], (), [accb])

    def resid_gemm(self, l):
        cfg = self.cfg
        A = self.A
        Dm, I = self.Dm, self.I
        D, CH, NCH = cfg.D, cfg.CH, cfg.NCH
        hT, hTb = Dm["hT"]
        state = {}

        def epi(ps, pb, gi, j, m0, msz, bi, blk):
            t0, size, r = blk
            if j == 0:
                ht, hb = self.tmpA if bi % 2 == 0 else self.tmpB
                hv = ht[:, 0:NCH * 512].rearrange("p (j t) -> p j t", t=512)
                self.dma(hv[:, :, 0:size], hT[:, t0:t0 + size].rearrange("(j p) t -> p j t", p=P), [hTb], [hb])
                state["h"] = (hv, hb)
            hv, hb = state["h"]
            g = self.mod(l, 2, j, r)
            A("dve", lambda e: e.scalar_tensor_tensor(out=hv[:, j, 0:size], in0=ps, scalar=g, in1=hv[:, j, 0:size], op0=ALU.mult, op1=ALU.add),
              [pb, self.modv[1]], (), [hb])

        def post(gi, g0, mg, bi, blk):
            t0, size, r = blk
            hv, hb = state["h"]
            self.dma(hT[:, t0:t0 + size].rearrange("(j p) t -> p j t", p=P), hv[:, :, 0:size], [hb], (), [hTb])

        self.gemm((I["w_out"][0][l * D:(l + 1) * D, :], I["w_out"][1]), D, CH,
                  lambda t0, size, kch: self.load_act(Dm["mg_f"], t0, size, kch), cfg.gblocks(), epi, post)

    def ffn_phase(self, l):
        cfg = self.cfg
        A = self.A
        Dm, I = self.Dm, self.I
        D, CH, NCH, T = cfg.D, cfg.CH, cfg.NCH, cfg.T
        moe = (l % 2 == 1)
        i = l // 2
        if moe:
            F = cfg.FE
            self.moe_gates(i)
            for k in range(2):
                W13 = (I["m_w13"][0][(2 * i + k) * D:(2 * i + k + 1) * D, :], I["m_w13"][1])
                W2 = (I["m_w2"][0][(2 * i + k) * F:(2 * i + k + 1) * F, :], I["m_w2"][1])
                self.ffn_core(W13, W2, F, gate_row=k, accumulate=(k == 1))
        else:
            F = cfg.FD
            W13 = (I["f_w13"][0][i * D:(i + 1) * D, :], I["f_w13"][1])
            W2 = (I["f_w2"][0][i * F:(i + 1) * F, :], I["f_w2"][1])
            self.ffn_core(W13, W2, F, gate_row=None, accumulate=False)
        for (t0_, sz_, ap_s, buf_s), (_, _, ap_f, buf_f) in zip(Dm["y_in"].items, Dm["y_out"].items):
            self.collective("ReduceScatter", (ap_s, buf_s), (ap_f, buf_f))
        hT, hTb = Dm["hT"]
        for bi, (t0, size, r) in enumerate(cfg.gblocks()):
            ht, hb = self.tmpA if bi % 2 == 0 else self.tmpB
            hv = ht[:, 0:NCH * 512].rearrange("p (j t) -> p j t", t=512)
            yt, yb = self.tmpC
            yv = yt[:, 0:NCH * 512].rearrange("p (j t) -> p j t", t=512)
            self.dma(hv[:, :, 0:size], hT[:, t0:t0 + size].rearrange("(j p) t -> p j t", p=P), [hTb], [hb])
            yo_ap, yo_buf, yb0 = Dm["y_out"].get(t0)
            self.dma(yv[:, :, 0:size], yo_ap[:, t0 - yb0:t0 - yb0 + size].rearrange("(j p) t -> p j t", p=P), [yo_buf], [yb])
            for j in range(NCH):
                g = self.mod(l, 5, j, r)
                A("dve", lambda e, hv=hv, yv=yv, j=j, g=g, size=size: e.scalar_tensor_tensor(
                    out=hv[:, j, 0:size], in0=yv[:, j, 0:size], scalar=g, in1=hv[:, j, 0:size], op0=ALU.mult, op1=ALU.add),
                  [yb, hb, self.modv[1]], (), [hb])
            self.dma(hT[:, t0:t0 + size].rearrange("(j p) t -> p j t", p=P), hv[:, :, 0:size], [hb], (), [hTb])

    def ffn_core(self, W13, W2, F, gate_row, accumulate):
        cfg = self.cfg
        A = self.A
        Dm = self.Dm
        D = cfg.D
        gb = cfg.gblocks()
        hid, hidb = Dm["hid"]
        state = {}

        def epi(ps, pb, gi, j, m0, msz, bi, blk):
            t0, size, r = blk
            if j == 0:
                state["o"] = self.rot("oh", self.obuf16)
            ot, ob = state["o"]
            ov = ot[:, :].rearrange("p (j t) -> p j t", t=512)
            st_, sb_ = self.h16
            if j % 2 == 0:
                A("act", lambda e: e.activation(out=st_[:, 0:size], in_=ps, func=AF.Silu), [pb], [sb_])
            else:
                A("dve", lambda e: e.tensor_tensor(out=ov[:, j // 2, 0:size], in0=ps, in1=st_[:, 0:size], op=ALU.mult),
                  [pb, sb_], [ob] if j == 1 else [], [] if j == 1 else [ob])

        def post(gi, g0, mg, bi, blk):
            t0, size, r = blk
            ot, ob = state["o"]
            ov = ot[:, :].rearrange("p (j t) -> p j t", t=512)
            nj = mg // (2 * P)
            f0 = g0 // 2
            self.dma(hid[f0:f0 + nj * P, t0:t0 + size].rearrange("(j p) t -> p j t", p=P), ov[:, 0:nj, 0:size], [ob], (), [hidb])

        self.gemm(W13, D, 2 * F, lambda t0, size, kch: self.load_act(Dm["xn_f"], t0, size, kch), gb, epi, post)
        kch = kchunks(F)
        nkc = len(kch)
        tw = 512 if nkc * 512 <= self.AB else 256
        blocks2 = []
        for (t0, size, r) in gb:
            t = t0
            while t < t0 + size:
                s = min(tw, t0 + size - t)
                blocks2.append((t, s, r))
                t += s
        st2 = {}

        def epi2(ps, pb, gi, j, m0, msz, bi, blk):
            t0, size, r = blk
            if j == 0:
                st2["o"] = self.rot("o", self.obuf)
                st2["g0"] = m0
                if gate_row is not None:
                    gt, gtb = self.tmpD
                    self.dma(gt[:, 0:size], Dm["gvec"][0][gate_row:gate_row + 1, t0:t0 + size].partition_broadcast(P), [Dm["gvec"][1]], [gtb])
                    st2["g"] = (gt, gtb)
                if accumulate:
                    pt_, ptb = self.tmpC
                    pv = pt_[:, :].rearrange("p (j t) -> p j t", t=512)
                    st2["p"] = (pv, ptb)
            ot, ob = st2["o"]
            ov = ot[:, :].rearrange("p (j t) -> p j t", t=512)
            if accumulate:
                pv, ptb = st2["p"]
                g0_ = m0
                y_in, y_inb, yb0 = Dm["y_in"].get(t0)
                self.dma(pv[0:msz, j, 0:size], y_in[g0_:g0_ + msz, t0 - yb0:t0 - yb0 + size], [y_inb], [ptb] if j == 0 else [], [] if j == 0 else [ptb])
            if gate_row is not None:
                gt, gtb = st2["g"]
                A("dve", lambda e: e.tensor_tensor(out=ov[0:msz, j, 0:size], in0=ps, in1=gt[0:msz, 0:size], op=ALU.mult),
                  [pb, gtb], [ob] if j == 0 else [], [] if j == 0 else [ob])
                if accumulate:
                    A("dve", lambda e: e.tensor_tensor(out=ov[0:msz, j, 0:size], in0=ov[0:msz, j, 0:size], in1=pv[0:msz, j, 0:size], op=ALU.add),
                      [ptb, ob], (), [ob])
            else:
                A("act", lambda e: e.activation(out=ov[0:msz, j, 0:size], in_=ps, func=AF.Copy),
                  [pb], [ob] if j == 0 else [], [] if j == 0 else [ob])

        def post2(gi, g0, mg, bi, blk):
            t0, size, r = blk
            ot, ob = st2["o"]
            ov = ot[:, :].rearrange("p (j t) -> p j t", t=512)
            nj = mg // P
            y_in, y_inb, yb0 = Dm["y_in"].get(t0)
            self.dma(y_in[g0:g0 + mg, t0 - yb0:t0 - yb0 + size].rearrange("(j p) t -> p j t", p=P), ov[:, 0:nj, 0:size], [ob], (), [y_inb])

        self.gemm(W2, F, D, lambda t0, size, kch: self.load_act(Dm["hid"], t0, size, kch), blocks2, epi2, post2, mg_max=512, combined=(F > 2048 or bool(os.environ.get("MK_FORCE_COMBINED"))))

    def moe_gates(self, i):
        cfg = self.cfg
        A = self.A
        Dm, I = self.Dm, self.I
        T = cfg.T
        n = T // P
        assert n * 8 <= 4 * 512 and 8 * n <= 4 * 512
        self.collective("AllReduce", Dm["lg_in"], Dm["lg_out"])
        lg, lgb = self.tmpA
        lv = lg[:, 0:n * 8].rearrange("p (n e) -> p n e", e=8)
        self.dma(lv, Dm["lg_out"][0].rearrange("(n p) e -> p n e", p=P), [Dm["lg_out"][1]], [lgb])
        w, wb = self.tmpB
        mk, mkb = self.tmpC
        mv = mk[:, 0:n * 8].rearrange("p (n e) -> p n e", e=8)
        m1 = w[:, 0:n]
        m2 = w[:, n:2 * n]
        g1 = w[:, 2 * n:3 * n]
        g2 = w[:, 3 * n:4 * n]
        accs = [w[:, 4 * n:5 * n], w[:, 7 * n:8 * n]]
        t1 = w[:, 5 * n:6 * n]
        t2 = w[:, 6 * n:7 * n]
        A("dve", lambda e: e.tensor_reduce(out=m1, in_=lv, axis=AX.X, op=ALU.max), [lgb], [wb])
        for e_ in range(8):
            A("dve", lambda e, e_=e_: e.tensor_tensor(out=t1, in0=lv[:, :, e_], in1=m1, op=ALU.is_equal), [lgb, wb], (), [wb])
            A("dve", lambda e, e_=e_: e.scalar_tensor_tensor(out=mv[:, :, e_], in0=t1, scalar=-1e30, in1=lv[:, :, e_], op0=ALU.mult, op1=ALU.add),
              [lgb, wb], [mkb] if e_ == 0 else [], [] if e_ == 0 else [mkb])
        A("dve", lambda e: e.tensor_reduce(out=m2, in_=mv, axis=AX.X, op=ALU.max), [mkb, wb], (), [wb])
        A("dve", lambda e: e.tensor_tensor(out=t1, in0=m2, in1=m1, op=ALU.subtract), [wb], (), [wb])
        A("act", lambda e: e.activation(out=t1, in_=t1, func=AF.Exp), [wb], (), [wb])
        A("dve", lambda e: e.tensor_scalar(out=t2, in0=t1, scalar1=1.0, scalar2=None, op0=ALU.add), [wb], (), [wb])
        A("dve", lambda e: e.reciprocal(out=g1, in_=t2), [wb], (), [wb])
        A("dve", lambda e: e.tensor_tensor(out=g2, in0=t1, in1=g1, op=ALU.mult), [wb], (), [wb])
        sel0 = self.vcols["sel"]
        vt, vb = self.vec
        for e_ in range(8):
            A("dve", lambda e, e_=e_: e.tensor_tensor(out=t1, in0=lv[:, :, e_], in1=m1, op=ALU.is_equal), [lgb, wb], (), [wb])
            A("dve", lambda e: e.tensor_tensor(out=t1, in0=t1, in1=g1, op=ALU.mult), [wb], (), [wb])
            A("dve", lambda e, e_=e_: e.tensor_tensor(out=t2, in0=mv[:, :, e_], in1=m2, op=ALU.is_equal), [mkb, wb], (), [wb])
            A("dve", lambda e: e.tensor_tensor(out=t2, in0=t2, in1=g2, op=ALU.mult), [wb], (), [wb])
            A("dve", lambda e: e.tensor_tensor(out=t1, in0=t1, in1=t2, op=ALU.add), [wb], (), [wb])
            for k in range(2):
                s = vt[:, 0, sel0 + 8 * k + e_:sel0 + 8 * k + e_ + 1]
                acc = accs[k]
                if e_ == 0:
                    A("dve", lambda e, s=s, acc=acc: e.tensor_scalar(out=acc, in0=t1, scalar1=s, scalar2=None, op0=ALU.mult), [wb, vb], (), [wb])
                else:
                    A("dve", lambda e, s=s, acc=acc: e.scalar_tensor_tensor(out=acc, in0=t1, scalar=s, in1=acc, op0=ALU.mult, op1=ALU.add), [wb, vb], (), [wb])
        first = True
        for k in range(2):
            gvd = Dm["gvec"][0][k, :].rearrange("(n p) -> p n", p=P)
            acc = accs[k]
            for n0 in range(0, n, 16):
                n1 = min(n, n0 + 16)
                self.A("sp", lambda e, n0=n0, n1=n1, gvd=gvd, acc=acc: e.dma_start(out=gvd[:, n0:n1], in_=acc[:, n0:n1], allow_slow_non_contiguous=True),
                       [wb], [Dm["gvec"][1]] if first else [], [] if first else [Dm["gvec"][1]], dma=True)
                first = False


IN_SIZES = (512, 256, 64, 512, 256, 256, 512, 512, 512, 512, 512, 512)


def rope_perm(dim):
    q = dim // 4
    return np.concatenate([np.arange(q, 2 * q), np.arange(0, q), np.arange(3 * q, 4 * q), np.arange(2 * q, 3 * q)])


def rope_tables(cfg, dim):
    n = cfg.SEQ
    pos = np.arange(n, dtype=np.int32)
    row = (pos // cfg.GW).astype(np.float32)
    col = (pos % cfg.GW).astype(np.float32)
    quarter = dim // 4
    inv_freq = (np.float32(10000.0) ** (-np.arange(quarter, dtype=np.float32) / np.float32(quarter))).astype(np.float32)
    ang_r = row[:, None] * inv_freq
    ang_c = col[:, None] * inv_freq
    ang = np.concatenate([ang_r, ang_r, ang_c, ang_c], axis=-1).astype(np.float32)
    cos = np.cos(ang).astype(np.float32)
    sin = np.sin(ang).astype(np.float32)
    sign = np.concatenate([-np.ones(quarter), np.ones(quarter), -np.ones(quarter), np.ones(quarter)]).astype(np.float32)
    sin = sin * sign[None, :]
    cosT = np.ones((dim, cfg.TB), np.float32)
    sinT = np.zeros((dim, cfg.TB), np.float32)
    cosT[:, cfg.CTX:] = cos.T
    sinT[:, cfg.CTX:] = sin.T
    return cosT, sinT


def prep_inputs(cfg, B, inp):
    L, D, CH, NCH, TB, T = cfg.DEPTH, cfg.D, cfg.CH, cfg.NCH, cfg.TB, cfg.T
    f = lambda a: np.ascontiguousarray(np.asarray(a, dtype=np.float32))
    x, c, ctx, c_ctx = f(inp["x"]), f(inp["c"]), f(inp["ctx"]), f(inp["c_ctx"])
    w_in = f(inp["w_in"])
    off = np.cumsum((0,) + IN_SIZES)
    gate0 = off[-1]
    cos64, sin64 = rope_tables(cfg, 64)
    cos128, sin128 = rope_tables(cfg, 128)
    rope_a = np.concatenate([np.tile(cos64, (2, 1)), np.tile(sin64, (2, 1)), cos128, sin128], axis=0)
    p64 = rope_perm(64)
    p128 = rope_perm(128)
    p64x2 = np.concatenate([p64, 64 + p64])
    nna = B.na_mask.shape[0]
    vcols, NV = B.vcols, B.NV
    mod_w = f(inp["mod_w"])
    shared = {}
    for i in range(G):
        ch = slice(i * CH, (i + 1) * CH)
        m = {}
        vecs = np.zeros((L, P, NV), np.float32)
        for l in range(L):
            for j in range(NCH):
                sl = slice(i * CH + j * P, i * CH + (j + 1) * P)
                vecs[l, :, vcols["n1g"] + j] = inp["norm1_g"][l][sl]
                vecs[l, :, vcols["n2g"] + j] = inp["norm2_g"][l][sl]
                vecs[l, :, vcols["fng"] + j] = inp["final_norm_g"][sl]
                for part in range(6):
                    vecs[l, :, vcols["modb"] + part * NCH + j] = inp["mod_b"][l][part * D + i * CH + j * P: part * D + i * CH + (j + 1) * P]
            for j in range(4):
                vecs[l, :, vcols["qng"] + j] = inp["mla_q_norm"][l][j * P:(j + 1) * P]
            for j in range(2):
                vecs[l, :, vcols["kvng"] + j] = inp["mla_kv_norm"][l][j * P:(j + 1) * P]
            vecs[l, :, vcols["sink"]] = inp["swa_sink"][l][i]
            vecs[l, :, vcols["subg"]] = inp["diff_subln_g"][l]
            vecs[l, :, vcols["lam4"]:vcols["lam4"] + 256] = np.asarray(inp["diff_lambda"][l]).reshape(1, 256)
            vecs[l, :, vcols["sel"] + 2 * i] = 1.0
            vecs[l, :, vcols["sel"] + 8 + 2 * i + 1] = 1.0
        m["vecs"] = vecs.reshape(L * P, NV)
        m["mod_w"] = np.concatenate([mod_w[:, :, part * D + i * CH: part * D + (i + 1) * CH] for part in range(6)], axis=2).reshape(L * D, 6 * CH)
        m["w_gate"] = np.concatenate([w_in[:, :, gate0 + mm * D + i * CH: gate0 + mm * D + (i + 1) * CH] for mm in range(4)], axis=2).reshape(L * D, 4 * CH)
        cq = w_in[:, :, off[0]:off[1]]
        ckv = w_in[:, :, off[1]:off[2]]
        kr = w_in[:, :, off[2]:off[3]]
        sq = w_in[:, :, off[3] + i * P: off[3] + (i + 1) * P]
        sk = w_in[:, :, off[4] + (i // 2) * P: off[4] + (i // 2 + 1) * P]
        sv = w_in[:, :, off[5] + (i // 2) * P: off[5] + (i // 2 + 1) * P]
        nq_ = w_in[:, :, off[6] + i * P: off[6] + (i + 1) * P]
        nk_ = w_in[:, :, off[7] + i * P: off[7] + (i + 1) * P]
        nv_ = w_in[:, :, off[8] + i * P: off[8] + (i + 1) * P]
        dq = w_in[:, :, off[9] + i * P: off[9] + (i + 1) * P]
        dk = w_in[:, :, off[10] + i * P: off[10] + (i + 1) * P]
        dv = w_in[:, :, off[11] + i * P: off[11] + (i + 1) * P]
        m["w_qkv"] = np.concatenate([cq, ckv, kr, kr[:, :, p64], sq, sq[:, :, p128], sk, sk[:, :, p128], nq_, nk_,
                                     dq, dq[:, :, p64x2], dk, dk[:, :, p64x2]], axis=2).reshape(L * D, cfg.NQKV)
        m["w_v"] = np.concatenate([sv, nv_, dv], axis=2).reshape(L * D, 3 * P)
        wuq = f(inp["mla_w_uq"])[:, :, i * 192:(i + 1) * 192]
        m["w_uq"] = np.concatenate([wuq[:, :, 0:128], wuq[:, :, 128:192], wuq[:, :, 128:192][:, :, p64]], axis=2).reshape(L * 512, 256)
        m["w_ukv"] = np.ascontiguousarray(f(inp["mla_w_ukv"])[:, :, i * 256:(i + 1) * 256]).reshape(L * 256, 256)
        wbr = f(inp["w_branch"])[:, :, :, ch]
        wbr = wbr.reshape(L, 4, 4, P, CH).transpose(0, 2, 1, 3, 4)
        m["w_br"] = np.ascontiguousarray(wbr).reshape(L * 2048, CH)
        m["w_out"] = np.ascontiguousarray(f(inp["w_out"])[:, :, ch]).reshape(L * D, CH)
        FDr = cfg.DFF // G
        w1 = np.zeros((cfg.ND, D, cfg.FD), np.float32)
        w3 = np.zeros((cfg.ND, D, cfg.FD), np.float32)
        w2 = np.zeros((cfg.ND, cfg.FD, D), np.float32)
        w1[:, :, :FDr] = inp["ffn_w1"][:, :, i * FDr:(i + 1) * FDr]
        w3[:, :, :FDr] = inp["ffn_w3"][:, :, i * FDr:(i + 1) * FDr]
        w2[:, :FDr, :] = inp["ffn_w2"][:, i * FDr:(i + 1) * FDr, :]
        w13 = np.stack([w1.reshape(cfg.ND, D, cfg.FD // P, P), w3.reshape(cfg.ND, D, cfg.FD // P, P)], axis=3)
        m["f_w13"] = w13.reshape(cfg.ND * D, 2 * cfg.FD)
        m["f_w2"] = w2.reshape(cfg.ND * cfg.FD, D)
        if cfg.NM:
            e1 = np.zeros((cfg.NM, 2, D, cfg.FE), np.float32)
            e3 = np.zeros((cfg.NM, 2, D, cfg.FE), np.float32)
            e2 = np.zeros((cfg.NM, 2, cfg.FE, D), np.float32)
            e1[:, :, :, :cfg.DFF] = inp["moe_w1"][:, 2 * i:2 * i + 2]
            e3[:, :, :, :cfg.DFF] = inp["moe_w3"][:, 2 * i:2 * i + 2]
            e2[:, :, :cfg.DFF, :] = inp["moe_w2"][:, 2 * i:2 * i + 2]
            e13 = np.stack([e1.reshape(cfg.NM, 2, D, cfg.FE // P, P), e3.reshape(cfg.NM, 2, D, cfg.FE // P, P)], axis=4)
            m["m_w13"] = e13.reshape(cfg.NM * 2 * D, 2 * cfg.FE)
            m["m_w2"] = e2.reshape(cfg.NM * 2 * cfg.FE, D)
            m["router"] = np.ascontiguousarray(f(inp["moe_router"])[:, ch, :]).reshape(cfg.NM * CH, 8)
        m["rope_a"] = rope_a
        m["swa_m"] = B.swa_mask.reshape(-1, 512)
        m["na_m"] = B.na_mask.reshape(-1, 512)
        rpb = f(inp["na_rpb"])
        m["na_b"] = np.ascontiguousarray(rpb[:, i][:, B.na_dr, B.na_dc]).reshape(L * nna * P, 512)
        shared[i] = m
    maps = []
    for core in range(NCORES):
        g, i = core // G, core % G
        m = dict(shared[i])
        tok = np.concatenate([ctx[g], x[g]], axis=0)
        m["xT"] = np.ascontiguousarray(tok[:, i * CH:(i + 1) * CH].T)
        m["cT"] = np.ascontiguousarray(np.stack([c[g], c_ctx], axis=1))
        maps.append(m)
    return maps


_CACHE = {}


def run(cfg, inputs, debug=(), stop=999):
    key = (cfg.D, cfg.SEQ, cfg.CTX, cfg.DFF, cfg.DEPTH, tuple(debug), stop)
    if key not in _CACHE:
        B = Builder(cfg, debug=debug, stop=stop)
        B.pslots = [Buf(f"pslot{i}") for i in range(8)]
        B.build()
        _CACHE[key] = B
    B = _CACHE[key]
    maps = prep_inputs(cfg, B, inputs)
    for m in maps:
        for k, (shape, dt) in B.inputs.items():
            assert tuple(m[k].shape) == shape, (k, m[k].shape, shape)
            if m[k].dtype != np.float32 or not m[k].flags["C_CONTIGUOUS"]:
                m[k] = np.ascontiguousarray(m[k], dtype=np.float32)
    res = run_bass_kernel_spmd(B.nc, maps, core_ids=list(range(NCORES)))
    if stop < 999:
        return None, res
    TB, CTX = cfg.TB, cfg.CTX
    outs = []
    for g in range(2):
        outT = np.concatenate([res.results[g * G + i]["outT"] for i in range(G)], axis=0)
        outs.append(outT.T[CTX:TB])
    out = np.stack(outs, axis=0)
    return np.ascontiguousarray(out.astype(np.float32)), res


def kernel(**inputs):
    cfg = Cfg()
    out, _ = run(cfg, inputs)
    return out
```

```python
import math
import numpy as np
from contextlib import ExitStack
import concourse.bass as bass
import concourse.mybir as mybir
from concourse.bass_utils import run_bass_kernel_spmd

F32 = mybir.dt.float32
BF16 = mybir.dt.bfloat16
ALU = mybir.AluOpType
AF = mybir.ActivationFunctionType
AX = mybir.AxisListType
P = 128
NCORES = 8
G = 4
RGROUPS = [[0, 1, 2, 3], [4, 5, 6, 7]]
NR = 2
import os
FAKECC = bool(os.environ.get("MK_FAKECC"))
COMPUTE = ("pe", "act", "dve", "pool")


ALL_BUFS = []


class _Stop(Exception):
    pass


class Buf:
    __slots__ = ("name", "w_eng", "w_dma", "r_eng", "r_dma")

    def __init__(self, name=""):
        ALL_BUFS.append(self)
        self.name = name
        self.w_eng = {}
        self.w_dma = []
        self.r_eng = {}
        self.r_dma = []


class Op:
    __slots__ = ("eng", "fn", "deps", "signaled", "sem", "val", "is_dma")

    def __init__(self, eng, fn, is_dma):
        self.eng = eng
        self.fn = fn
        self.deps = []
        self.signaled = False
        self.sem = None
        self.val = 0
        self.is_dma = is_dma


class Sched:
    def __init__(self, nc, n_dma_sems=12, n_cc_sems=64):
        self.nc = nc
        self.ops = {e: [] for e in ("pe", "act", "dve", "pool", "sp")}
        self.n_dma_sems = n_dma_sems
        self.n_cc_sems = n_cc_sems

    def op(self, eng, fn, reads=(), writes=(), adds=(), dma=False, cc=False):
        o = Op(eng, fn, "cc" if cc else bool(dma))
        if cc:
            o.signaled = True
        deps = {}

        def add(d):
            if d is not o:
                deps[id(d)] = d

        for b in reads:
            for d in b.w_eng.values():
                add(d)
            for d in b.w_dma:
                add(d)
        for b in writes:
            for d in b.w_eng.values():
                add(d)
            for d in b.w_dma:
                add(d)
            for d in b.r_eng.values():
                add(d)
            for d in b.r_dma:
                add(d)
        for b in adds:
            for d in b.r_eng.values():
                add(d)
            for d in b.r_dma:
                add(d)
            for e2, d in b.w_eng.items():
                if o.is_dma or e2 != eng:
                    add(d)
            if not o.is_dma:
                for d in b.w_dma:
                    add(d)
        dl = []
        for d in deps.values():
            if (not d.is_dma) and (not o.is_dma) and d.eng == eng == "pe":
                continue
            d.signaled = True
            dl.append(d)
        o.deps = dl
        for b in writes:
            b.w_eng = {}
            b.w_dma = []
            b.r_eng = {}
            b.r_dma = []
        for b in list(writes) + list(adds):
            if o.is_dma:
                b.w_dma.append(o)
            else:
                b.w_eng[eng] = o
        for b in reads:
            if o.is_dma:
                b.r_dma.append(o)
            else:
                b.r_eng[eng] = o
        self.ops[eng].append(o)
        return o

    def emit(self):
        nc = self.nc
        with ExitStack() as st:
            esem = {e: st.enter_context(nc.semaphore(f"s_{e}")) for e in COMPUTE}
            dsem = {q: [st.enter_context(nc.semaphore(f"d_{q}{i}")) for i in range(self.n_dma_sems)]
                    for q in ("sp", "pool")}
            ccsem = st.enter_context(nc.semaphore("ccsem"))
            cci = 0
            for e, lst in self.ops.items():
                cnt = 0
                dcnt = 0
                for o in lst:
                    if not o.signaled:
                        continue
                    if o.is_dma == "cc":
                        cci += 1
                        o.sem = ccsem
                        o.val = cci
                    elif o.is_dma:
                        n = self.n_dma_sems
                        o.sem = dsem[e][dcnt % n]
                        o.val = 16 * (dcnt // n + 1)
                        dcnt += 1
                    else:
                        cnt += 1
                        o.sem = esem[e]
                        o.val = cnt
            import os
            if os.environ.get("MK_VERBOSE"):
                for e, lst in self.ops.items():
                    sig = [o for o in lst if o.signaled]
                    print("ENG", e, "ops", len(lst), "signaled", len(sig), "maxval", max([o.val for o in sig] + [0]), flush=True)
            block = st.enter_context(nc.Block())

            def run(eng_name):
                def body(e):
                    waited = {}
                    for o in self.ops[eng_name]:
                        need = {}
                        for d in o.deps:
                            k = id(d.sem)
                            if k not in need or need[k][1] < d.val:
                                need[k] = (d.sem, d.val)
                        for k, (s, v) in need.items():
                            if waited.get(k, 0) >= v:
                                continue
                            e.wait_ge(s, v)
                            waited[k] = v
                        ins = o.fn(e)
                        if o.signaled:
                            ins.then_inc(o.sem, 16 if o.is_dma is True else 1)
                return body

            block.tensor(run("pe"))
            block.scalar(run("act"))
            block.vector(run("dve"))
            block.gpsimd(run("pool"))
            block.sync(run("sp"))


class Cfg:
    def __init__(self, D=2048, SEQ=4096, CTX=256, DFF=5632, DEPTH=4):
        self.D, self.SEQ, self.CTX, self.DFF, self.DEPTH = D, SEQ, CTX, DFF, DEPTH
        self.GW = 64
        self.TB = SEQ + CTX
        self.T = self.TB
        self.CH = D // G
        assert self.CH % P == 0
        self.NCH = self.CH // P
        self.KD = D // P
        self.FD = ((DFF // G) + P - 1) // P * P
        self.FE = (DFF + P - 1) // P * P
        self.NE = 8
        self.ND = (DEPTH + 1) // 2
        self.NM = DEPTH // 2
        self.NQKV = 17 * P
        self.eps = 1e-6

    def lblocks(self):
        out = []
        t = 0
        while t < self.CTX:
            s = min(512, self.CTX - t)
            out.append((t, s, "ctx"))
            t += s
        while t < self.TB:
            s = min(512, self.TB - t)
            out.append((t, s, "lat"))
            t += s
        return out

    def gblocks(self):
        return [(t0, s, 0 if kind == "lat" else 1) for (t0, s, kind) in self.lblocks()]


def kchunks(K):
    out = []
    k = 0
    while k < K:
        out.append((k, min(P, K - k)))
        k += P
    return out


class Blk:
    def __init__(self, builder, name, rows, dt, blocks):
        self.items = []
        for i, blk in enumerate(blocks):
            t0, size = blk[0], blk[1]
            ap, buf = builder.dram(f"{name}_{i}", [rows, size], dt)
            self.items.append((t0, size, ap, buf))

    def get(self, t):
        for (t0, size, ap, buf) in self.items:
            if t0 <= t < t0 + size:
                return ap, buf, t0
        raise KeyError(t)


def vec_cols(cfg):
    cols = {}
    n = 0

    def add(name, cnt):
        nonlocal n
        cols[name] = n
        n += cnt

    add("n1g", cfg.NCH)
    add("n2g", cfg.NCH)
    add("fng", cfg.NCH)
    add("modb", 6 * cfg.NCH)
    add("qng", 4)
    add("kvng", 2)
    add("sink", 1)
    add("subg", 1)
    add("lam4", 4 * 64)
    add("sel", 16)
    return cols, n


def na_schedule(cfg):
    rows = cfg.SEQ // cfg.GW
    KH, KW, GW = 8, 16, cfg.GW
    kh = min(KH, rows)
    r = np.arange(rows)
    row_start = np.clip(r - kh // 2, 0, rows - kh)
    col = np.arange(GW)
    col_start = np.clip(col - KW // 2, 0, GW - KW)
    nq = cfg.SEQ // 512
    uniq = {}
    masks, drs, dcs = [], [], []
    sched = []
    for Q in range(nq):
        qr = np.arange(8 * Q, 8 * Q + 8)
        kr_lo = row_start[qr].min()
        kr_hi = row_start[qr].max() + kh - 1
        lst = []
        for kt in range(kr_lo // 2, kr_hi // 2 + 1):
            kr = np.array([2 * kt, 2 * kt + 1])
            KR = np.repeat(kr, GW)[:, None]
            KC = np.tile(col, 2)[:, None]
            QR = np.repeat(qr, GW)[None, :]
            QC = np.tile(col, 8)[None, :]
            rv = (KR >= row_start[QR]) & (KR < row_start[QR] + kh) & (KR < rows)
            cv = (KC >= col_start[QC]) & (KC < col_start[QC] + KW)
            m = (rv & cv)
            dr = np.clip(KR - QR + (KH - 1), 0, 2 * KH - 2) + 0 * QC
            dc = np.clip(KC - QC, -(KW - 1), KW - 1) + (KW - 1) + 0 * QR
            if not m.any():
                continue
            key = (m.tobytes(), (dr * m).tobytes(), (dc * m).tobytes())
            if key not in uniq:
                uniq[key] = len(masks)
                masks.append(m.astype(np.float32))
                drs.append(dr.astype(np.int64))
                dcs.append(dc.astype(np.int64))
            lst.append((kt, uniq[key]))
        sched.append(lst)
    return sched, np.stack(masks), np.stack(drs), np.stack(dcs)


def swa_schedule(cfg):
    nq = cfg.SEQ // 512
    nkt = cfg.SEQ // P
    sched = []
    variants = {}
    masks = []
    for Q in range(nq):
        lst = []
        for kt in range(max(0, 4 * Q - 1), min(nkt, 4 * Q + 5)):
            k = kt * P + np.arange(P)[:, None]
            q = Q * 512 + np.arange(512)[None, :]
            m = (np.abs(k - q) <= 128)
            if m.all():
                lst.append((kt, None))
                continue
            key = m.tobytes()
            if key not in variants:
                variants[key] = len(masks)
                masks.append(m.astype(np.float32))
            lst.append((kt, variants[key]))
        sched.append(lst)
    return sched, np.stack(masks)


class Builder:
    WB = 16384
    AB = 11264

    def __init__(self, cfg, debug=(), stop=999):
        self.cfg = cfg
        self.stop = stop
        del ALL_BUFS[:]
        self.debug = set(debug)
        self.nc = bass.Bass("TRN2", target_bir_lowering=False)
        self.S = Sched(self.nc)
        self.st = ExitStack()
        nc = self.nc
        wall, _ = self.sb("wbuf_all", [P, 2 * self.WB], BF16)
        self.wall = wall
        self.wbuf = [(wall[:, i * self.WB:(i + 1) * self.WB], Buf(f"wbuf{i}")) for i in range(2)]
        self.abuf = [self.sb(f"abuf{i}", [P, self.AB], BF16) for i in range(2)]
        self.obuf = [self.sb(f"obuf{i}", [P, 4 * 512], F32) for i in range(2)]
        self.obuf16 = [self.sb(f"obh{i}", [P, 8 * 512], BF16) for i in range(2)]
        self.ps = []
        for i in range(8):
            t = self.st.enter_context(nc.psum_tensor(f"ps{i}", [P, 512], F32))
            self.ps.append((t, Buf(f"ps{i}")))
        self.ps_cnt = {}
        self.cnt = {"w": 0, "a": 0, "o": 0, "oh": 0}
        self.inputs = {}
        self.na_s, self.na_mask, self.na_dr, self.na_dc = na_schedule(cfg)
        self.swa_s, self.swa_mask = swa_schedule(cfg)
        self.vcols, self.NV = vec_cols(cfg)
        self._boff = {}

    def sb(self, name, shape, dt):
        t = self.st.enter_context(self.nc.sbuf_tensor(name, shape, dt))
        return (t, Buf(name))

    def dram(self, name, shape, dt):
        kind = "ExternalOutput" if name in self.debug else "Internal"
        t = self.nc.dram_tensor(name, shape, dt, kind=kind).ap()
        return (t, Buf(name))

    def inp(self, name, shape, dt=F32):
        t = self.nc.dram_tensor(name, shape, dt, kind="ExternalInput").ap()
        self.inputs[name] = (tuple(shape), dt)
        return (t, Buf(name))

    def psum(self, pool=(0, 1, 2, 3, 4, 5, 6, 7)):
        c = self.ps_cnt.get(pool, 0)
        self.ps_cnt[pool] = c + 1
        return self.ps[pool[c % len(pool)]]

    def rot(self, which, lst):
        r = lst[self.cnt[which] % len(lst)]
        self.cnt[which] += 1
        return r

    def A(self, eng, fn, r=(), w=(), a=(), dma=False, cc=False):
        return self.S.op(eng, fn, reads=r, writes=w, adds=a, dma=dma, cc=cc)

    def dma(self, out, in_, r, w=(), a=(), q="sp"):
        return self.A(q, lambda e: e.dma_start(out=out, in_=in_), r, w, a, dma=True)

    def boff(self, e, key):
        if key not in self._boff:
            self._boff[key] = (e.partition_id() // 4) * self.cfg.TB
        return self._boff[key]

    def load_weights(self, W, m0, mg, kch, dst=None, combined=False):
        Wap, Wbuf = W
        nkc = len(kch)
        if combined:
            assert nkc * mg <= 2 * self.WB, (nkc, mg)
            view = self.wall[:, 0:nkc * mg].rearrange("p (k m) -> p k m", m=mg)
            wbl = [self.wbuf[0][1], self.wbuf[1][1]]
        elif dst is None:
            wt, wb = self.rot("w", self.wbuf)
            assert nkc * mg <= self.WB, (nkc, mg)
            view = wt[:, 0:nkc * mg].rearrange("p (k m) -> p k m", m=mg)
            wbl = [wb]
        else:
            view, wb = dst
            wbl = [wb]
        nfull = sum(1 for (_, ks) in kch if ks == P)
        step = 4
        first = True
        for k0 in range(0, nfull, step):
            k1 = min(nfull, k0 + step)
            src = Wap[k0 * P:k1 * P, m0:m0 + mg].rearrange("(k p) m -> p k m", p=P)
            self.dma(view[:, k0:k1, :], src, [Wbuf], wbl if first else [], [] if first else wbl, q="pool")
            first = False
        if nfull < nkc:
            k0, ks = kch[-1]
            self.dma(view[0:ks, nfull, :], Wap[k0:k0 + ks, m0:m0 + mg], [Wbuf],
                     wbl if first else [], [] if first else wbl, q="pool")
        return view, wbl

    def load_act(self, A, t0, size, kch, row0=0, dyn=False, dst=None):
        nkc = len(kch)
        if dst is not None:
            at, ab = dst
            tw = 512
        else:
            at, ab = self.rot("a", self.abuf)
            tw = 512 if nkc * 512 <= self.AB else 256
            assert size <= tw and nkc * tw <= self.AB
        view = at[:, 0:nkc * tw].rearrange("p (k t) -> p k t", t=tw)
        if isinstance(A, Blk):
            Aap, Abuf, b0 = A.get(t0)
        else:
            (Aap, Abuf), b0 = A, 0
        nfull = sum(1 for (_, ks) in kch if ks == P)
        step = 4
        first = True

        def mk(dst, r0, r1, full):
            def fn(e):
                tsl = slice(t0 - b0, t0 - b0 + size)
                if full:
                    src = Aap[r0:r1, tsl].rearrange("(k p) t -> p k t", p=P)
                else:
                    src = Aap[r0:r1, tsl]
                return e.dma_start(out=dst, in_=src)
            return fn

        for k0 in range(0, nfull, step):
            k1 = min(nfull, k0 + step)
            self.A("sp", mk(view[:, k0:k1, 0:size], row0 + k0 * P, row0 + k1 * P, True), [Abuf],
                   [ab] if first else [], [] if first else [ab], dma=True)
            first = False
        if nfull < nkc:
            k0, ks = kch[-1]
            self.A("sp", mk(view[0:ks, nfull, 0:size], row0 + k0, row0 + k0 + ks, False), [Abuf],
                   [ab] if first else [], [] if first else [ab], dma=True)
        return view, ab

    def gemm(self, *a, **kw):
        for _ in self.gemm_g(*a, **kw):
            pass

    def gemm_g(self, W, K, M, act_loader, tblocks, epilogue, post_block=None, per_block=None, mg_max=1024, combined=False):
        kch = kchunks(K)
        nkc = len(kch)
        mg_cap = min(mg_max, ((2 * self.WB if combined else self.WB) // nkc) // P * P)
        groups = []
        m = 0
        while m < M:
            groups.append((m, min(mg_cap, M - m)))
            m += mg_cap
        iters = [(gi, bi) for gi in range(len(groups)) for bi in range(len(tblocks))]
        pending = act_loader(tblocks[0][0], tblocks[0][1], kch)
        for it, (gi, bi) in enumerate(iters):
            g0, mg = groups[gi]
            blk = tblocks[bi]
            if bi == 0:
                wv, wbl = self.load_weights(W, g0, mg, kch, combined=combined)
            if True:
                t0, size = blk[0], blk[1]
                av, ab = pending
                if per_block is not None and gi == 0:
                    per_block(av, ab, bi, blk)
                j = 0
                m0 = 0
                while m0 < mg:
                    msz = min(P, mg - m0)
                    pt, pb = self.psum()
                    for ki, (k0, ks) in enumerate(kch):
                        self.A("pe",
                               lambda e, pt=pt, wv=wv, av=av, ki=ki, ks=ks, m0=m0, msz=msz, size=size:
                               e.matmul(pt[0:msz, 0:size], wv[0:ks, ki, m0:m0 + msz], av[0:ks, ki, 0:size],
                                        start=(ki == 0), stop=(ki == nkc - 1)),
                               wbl + [ab], [pb] if ki == 0 else [], [] if ki == 0 else [pb])
                    epilogue(pt[0:msz, 0:size], pb, gi, j, g0 + m0, msz, bi, blk)
                    m0 += msz
                    j += 1
                if it + 1 < len(iters):
                    nblk = tblocks[iters[it + 1][1]]
                    pending = act_loader(nblk[0], nblk[1], kch)
                if post_block is not None:
                    post_block(gi, g0, mg, bi, blk)
                yield

    def std_epilogue(self, dst, func=AF.Copy, out_dt=BF16, row0=0, toff=0, eng="act"):
        state = {}
        dap, dbuf = dst

        def epi(ps, pb, gi, j, m0, msz, bi, blk):
            size = blk[1]
            if j == 0:
                state["o"] = self.rot("oh", self.obuf16) if out_dt == BF16 else self.rot("o", self.obuf)
            ot, ob = state["o"]
            ov = ot[:, :].rearrange("p (j t) -> p j t", t=512)
            if eng == "act":
                self.A("act", lambda e: e.activation(out=ov[0:msz, j, 0:size], in_=ps, func=func),
                       [pb], [ob] if j == 0 else [], [] if j == 0 else [ob])
            else:
                self.A("dve", lambda e: e.tensor_copy(out=ov[0:msz, j, 0:size], in_=ps),
                       [pb], [ob] if j == 0 else [], [] if j == 0 else [ob])

        def post(gi, g0, mg, bi, blk):
            t0, size = blk[0], blk[1]
            ot, ob = state["o"]
            ov = ot[:, :].rearrange("p (j t) -> p j t", t=512)
            nj = mg // P
            if nj > 0:
                d = dap[row0 + g0:row0 + g0 + nj * P, toff + t0:toff + t0 + size].rearrange("(j p) t -> p j t", p=P)
                self.dma(d, ov[:, 0:nj, 0:size], [ob], (), [dbuf])
            rem = mg - nj * P
            if rem:
                d = dap[row0 + g0 + nj * P:row0 + g0 + mg, toff + t0:toff + t0 + size]
                self.dma(d, ov[0:rem, nj, 0:size], [ob], (), [dbuf])

        return epi, post

    def collective(self, kind, src, dst, groups=None):
        sap, sbuf_ = src
        dap, dbuf = dst
        rg = groups or RGROUPS
        op = ALU.bypass if kind == "AllGather" else ALU.add
        if FAKECC:
            rows = sap.shape[0]
            if kind == "AllGather":
                for r in range(G):
                    self.dma(dap[r * rows:(r + 1) * rows, :], sap, [sbuf_], [dbuf] if r == 0 else [], [] if r == 0 else [dbuf])
            elif kind == "AllReduce":
                self.dma(dap, sap, [sbuf_], [dbuf])
            else:
                self.dma(dap, sap[0:dap.shape[0], :], [sbuf_], [dbuf])
            return
        self.A("pool", lambda e: e.collective_compute(kind, op, replica_groups=rg, ins=[sap], outs=[dap]),
               [sbuf_], [dbuf], cc=True)

    def build(self):
        cfg = self.cfg
        nc = self.nc
        L, D, T, TB, CH, NCH, KD = cfg.DEPTH, cfg.D, cfg.T, cfg.TB, cfg.CH, cfg.NCH, cfg.KD
        A = self.A
        I = {}
        I["xT"] = self.inp("xT", [CH, T])
        I["cT"] = self.inp("cT", [D, NR])
        I["vecs"] = self.inp("vecs", [L * P, self.NV])
        I["mod_w"] = self.inp("mod_w", [L * D, 6 * CH])
        I["w_gate"] = self.inp("w_gate", [L * D, 4 * CH])
        I["w_qkv"] = self.inp("w_qkv", [L * D, cfg.NQKV])
        I["w_v"] = self.inp("w_v", [L * D, 3 * P])
        I["w_uq"] = self.inp("w_uq", [L * 512, 256])
        I["w_ukv"] = self.inp("w_ukv", [L * 256, 256])
        I["w_br"] = self.inp("w_br", [L * 2048, CH])
        I["w_out"] = self.inp("w_out", [L * D, CH])
        I["f_w13"] = self.inp("f_w13", [cfg.ND * D, 2 * cfg.FD])
        I["f_w2"] = self.inp("f_w2", [cfg.ND * cfg.FD, D])
        if cfg.NM:
            I["m_w13"] = self.inp("m_w13", [cfg.NM * 2 * D, 2 * cfg.FE])
            I["m_w2"] = self.inp("m_w2", [cfg.NM * 2 * cfg.FE, D])
            I["router"] = self.inp("router", [cfg.NM * CH, 8])
        I["rope_a"] = self.inp("rope_a", [4 * P, TB])
        I["swa_m"] = self.inp("swa_m", [self.swa_mask.shape[0] * P, 512])
        I["na_m"] = self.inp("na_m", [self.na_mask.shape[0] * P, 512])
        I["na_b"] = self.inp("na_b", [L * self.na_mask.shape[0] * P, 512])
        self.I = I
        Dm = {}
        Dm["hT"] = self.dram("hT", [CH, T], F32)
        Dm["ss_in"] = self.dram("ss_in", [1, T], F32)
        Dm["ss_out"] = self.dram("ss_out", [1, T], F32)
        lbk = cfg.lblocks()
        Dm["xn_s"] = Blk(self, "xn_s", CH, BF16, lbk)
        Dm["xn_f"] = Blk(self, "xn_f", D, BF16, lbk)
        Dm["sg"] = self.dram("sg", [4 * CH, T], BF16)
        Dm["u"] = self.dram("u", [cfg.NQKV, TB], BF16)
        Dm["vtm"] = self.dram("vtm", [TB, 4 * P], BF16)
        Dm["qk"] = self.dram("qk", [12 * P, TB], BF16)
        Dm["o_s"] = Blk(self, "o_s", 4 * P, BF16, lbk)
        Dm["o_f"] = Blk(self, "o_f", G * 4 * P, BF16, lbk)
        Dm["mg_s"] = Blk(self, "mg_s", CH, BF16, lbk)
        Dm["mg_f"] = Blk(self, "mg_f", D, BF16, lbk)
        Dm["hid"] = self.dram("hid", [max(cfg.FD, cfg.FE if cfg.NM else 0), T], BF16)
        Dm["y_in"] = Blk(self, "y_in", D, F32, lbk)
        Dm["y_out"] = Blk(self, "y_out", CH, F32, lbk)
        if cfg.NM:
            Dm["lg_in"] = self.dram("lg_in", [T, 8], F32)
            Dm["lg_out"] = self.dram("lg_out", [T, 8], F32)
            Dm["gvec"] = self.dram("gvec", [2, T], F32)
        self.out = self.nc.dram_tensor("outT", [CH, T], F32, kind="ExternalOutput").ap()
        self.outb = Buf("outT")
        self.Dm = Dm
        self.ones16 = self.sb("ones16", [P, P], BF16)
        self.vec = self.sb("vec", [P, L, self.NV], F32)
        self.modv = self.sb("modv", [P, L * 6 * NCH * NR], F32)
        self.modA = self.sb("modA", [P, L * 4 * NCH * NR], F32)
        self.csil = self.sb("csil", [P, KD * 4], BF16)
        self.c32 = self.sb("c32", [P, KD * 4], F32)
        self.ssrow = [self.sb(f"ssrow{i}", [1, 512], F32) for i in range(2)]
        self.wv_sb = self.sb("wv_sb", [P, KD * 3 * P], BF16)
        self.wuq_sb = self.sb("wuq_sb", [P, 4 * 256], BF16)
        self.wukv_sb = self.sb("wukv_sb", [P, 2 * 256], BF16)
        self.tmpA = self.sb("tmpA", [P, 4 * 512], F32)
        self.tmpB = self.sb("tmpB", [P, 4 * 512], F32)
        self.tmpC = self.sb("tmpC", [P, 4 * 512], F32)
        self.tmpD = self.sb("tmpD", [P, 2 * 512], F32)
        self.h16 = self.sb("h16", [P, 4 * 512], BF16)
        self.vst = [self.sb(f"vst{i}", [P, 3 * P], BF16) for i in range(2)]
        self.mla_in = self.sb("mla_in", [P, 4 * 512], BF16)
        self.h16b = self.sb("h16b", [P, 4 * 512], BF16)
        self.lamv = self.sb("lamv", [P, 16], F32)
        self.lgst = [self.sb(f"lgst{i}", [P, 8], F32) for i in range(2)]
        self.rt_sb = self.sb("rt_sb", [P, NCH * 8], F32)

        ot, ob = self.ones16
        A("dve", lambda e: e.memset(ot[:, :], 1.0), (), [ob])
        vt, vb = self.vec
        self.dma(vt[:, :, :], I["vecs"][0].rearrange("(l p) n -> p l n", p=P), [I["vecs"][1]], [vb])
        self.dma(Dm["hT"][0][:, :], I["xT"][0][:, :], [I["xT"][1]], [Dm["hT"][1]])

        try:
            self.phase(1)
            self.emit_mod()
            for l in range(L):
                self.layer(l)
            self.phase(12)
            self.norm_phase(L - 1, which="final")
        except _Stop:
            pass
        A("sp", lambda e: e.nop(), list(ALL_BUFS))
        self.S.emit()
        return self.nc

    def phase(self, k):
        if k > self.stop:
            raise _Stop()

    def vcol(self, l, name, j=0):
        vt, vb = self.vec
        c = self.vcols[name] + j
        return vt[:, l, c:c + 1]

    def mod(self, l, part, j, r):
        mt, mb = self.modv
        idx = ((l * 6 + part) * self.cfg.NCH + j) * NR + r
        return mt[:, idx:idx + 1]

    def modA_(self, l, which, j, r):
        mt, mb = self.modA
        idx = ((l * 4 + which) * self.cfg.NCH + j) * NR + r
        return mt[:, idx:idx + 1]

    def emit_mod(self):
        cfg = self.cfg
        A = self.A
        KD, NCH, L, D, CH = cfg.KD, cfg.NCH, cfg.DEPTH, cfg.D, cfg.CH
        ct, cb = self.c32
        st_, sbf = self.csil
        cv = ct[:, 0:KD * NR].rearrange("p (k r) -> p k r", r=NR)
        sv = st_[:, 0:KD * NR].rearrange("p (k r) -> p k r", r=NR)
        self.dma(cv, self.I["cT"][0].rearrange("(k p) r -> p k r", p=P), [self.I["cT"][1]], [cb])
        A("act", lambda e: e.activation(out=sv, in_=cv, func=AF.Silu), [cb], [sbf])
        mt, mb = self.modv
        vt, vb = self.vec
        first = [True]
        for l in range(L):
            W = (self.I["mod_w"][0][l * D:(l + 1) * D, :], self.I["mod_w"][1])

            def loader(t0, size, kch):
                return sv, sbf

            def epi(ps, pb, gi, j, m0, msz, bi, blk, l=l):
                jj = m0 // P
                idx = (l * 6 * NCH + jj) * NR
                bcol = self.vcols["modb"] + jj
                A("dve", lambda e: e.tensor_scalar(out=mt[:, idx:idx + NR], in0=ps, scalar1=vt[:, l, bcol:bcol + 1],
                                                   scalar2=None, op0=ALU.add),
                  [pb, vb], [mb] if first[0] else [], [] if first[0] else [mb])
                first[0] = False

            self.gemm(W, D, 6 * CH, loader, [(0, NR)], epi)
        at, ab = self.modA
        firstA = True
        for l in range(L):
            for which, (gname, part) in enumerate((("n1g", 1), ("n2g", 4))):
                for j in range(NCH):
                    src = ((l * 6 + part) * NCH + j) * NR
                    dst = ((l * 4 + which) * NCH + j) * NR
                    g = self.vcol(l, gname, j)
                    A("dve", lambda e, src=src, dst=dst, g=g: e.tensor_scalar(
                        out=at[:, dst:dst + NR], in0=mt[:, src:src + NR], scalar1=1.0, scalar2=g,
                        op0=ALU.add, op1=ALU.mult),
                      [mb, vb], [ab] if firstA else [], [] if firstA else [ab])
                    firstA = False

    def norm_phase(self, l, which):
        cfg = self.cfg
        A = self.A
        NCH, T, D = cfg.NCH, cfg.T, cfg.D
        Dm = self.Dm
        hT, hTb = Dm["hT"]
        ones, onesb = self.ones16
        blocks = cfg.gblocks()
        moe = (which == 2 and l % 2 == 1)
        first = True
        for bi, (t0, size, r) in enumerate(blocks):
            ht, hb = self.tmpA if bi % 2 == 0 else self.tmpC
            hv = ht[:, 0:NCH * 512].rearrange("p (j t) -> p j t", t=512)
            self.dma(hv[:, :, 0:size], hT[:, t0:t0 + size].rearrange("(j p) t -> p j t", p=P), [hTb], [hb])
            qt, qb = self.h16 if bi % 2 == 0 else self.h16b
            qv = qt[:, 0:NCH * 512].rearrange("p (j t) -> p j t", t=512)
            A("act", lambda e, qv=qv, hv=hv, size=size: e.activation(out=qv[:, :, 0:size], in_=hv[:, :, 0:size], func=AF.Square),
              [hb], [qb])
            pt, pb = self.psum()
            for j in range(NCH):
                A("pe", lambda e, pt=pt, qv=qv, j=j, size=size: e.matmul(pt[0:1, 0:size], ones[:, 0:1], qv[:, j, 0:size],
                                                                         start=(j == 0), stop=(j == NCH - 1)),
                  [onesb, qb], [pb] if j == 0 else [], [] if j == 0 else [pb])
            srow, srowb = self.ssrow[self.cnt["o"] % 2]
            self.cnt["o"] += 1
            A("dve", lambda e, pt=pt, srow=srow, size=size: e.tensor_copy(out=srow[0:1, 0:size], in_=pt[0:1, 0:size]),
              [pb], [srowb])
            self.dma(Dm["ss_in"][0][0:1, t0:t0 + size], srow[0:1, 0:size], [srowb], [Dm["ss_in"][1]] if first else [], [] if first else [Dm["ss_in"][1]])
            first = False
        self.collective("AllReduce", Dm["ss_in"], Dm["ss_out"])
        for bi, (t0, size, r) in enumerate(blocks):
            bt, bb = self.tmpB if bi % 2 == 0 else self.tmpD
            self.dma(bt[:, 0:size], Dm["ss_out"][0][0:1, t0:t0 + size].partition_broadcast(P), [Dm["ss_out"][1]], [bb])
            A("act", lambda e, bt=bt, size=size: e.activation(out=bt[:, 0:size], in_=bt[:, 0:size], func=AF.Sqrt,
                                                              scale=1.0 / D, bias=cfg.eps), [bb], [bb])
            A("dve", lambda e, bt=bt, size=size: e.reciprocal(out=bt[:, 0:size], in_=bt[:, 0:size]), [bb], [bb])
            ht, hb = self.tmpA if bi % 2 == 0 else self.tmpC
            hv = ht[:, 0:NCH * 512].rearrange("p (j t) -> p j t", t=512)
            self.dma(hv[:, :, 0:size], hT[:, t0:t0 + size].rearrange("(j p) t -> p j t", p=P), [hTb], [hb])
            xt, xb = ht, hb
            xv = hv
            for j in range(NCH):
                if which == "final":
                    gain = self.vcol(0, "fng", j)
                    shift = None
                else:
                    gain = self.modA_(l, 0 if which == 1 else 1, j, r)
                    shift = self.mod(l, 0 if which == 1 else 3, j, r)
                A("dve", lambda e, xv=xv, hv=hv, bt=bt, j=j, size=size, gain=gain: e.scalar_tensor_tensor(
                    out=xv[:, j, 0:size], in0=hv[:, j, 0:size], scalar=gain, in1=bt[:, 0:size],
                    op0=ALU.mult, op1=ALU.mult),
                  [hb, bb, self.modA[1], self.vec[1]], (), [xb])
                if shift is not None:
                    A("dve", lambda e, xv=xv, j=j, size=size, shift=shift: e.tensor_scalar(
                        out=xv[:, j, 0:size], in0=xv[:, j, 0:size], scalar1=shift, scalar2=None, op0=ALU.add),
                      [xb, self.modv[1]], (), [xb])
            if which == "final":
                self.dma(self.out[:, t0:t0 + size].rearrange("(j p) t -> p j t", p=P), xv[:, :, 0:size], [xb], (), [self.outb])
                continue
            yt, yb = self.h16b if bi % 2 == 0 else self.h16
            yv = yt[:, 0:NCH * 512].rearrange("p (j t) -> p j t", t=512)
            A("act", lambda e, yv=yv, xv=xv, size=size: e.activation(out=yv[:, :, 0:size], in_=xv[:, :, 0:size], func=AF.Copy),
              [xb], [yb])
            xs_ap, xs_buf, xb0 = Dm["xn_s"].get(t0)
            self.dma(xs_ap[:, t0 - xb0:t0 - xb0 + size].rearrange("(j p) t -> p j t", p=P), yv[:, :, 0:size], [yb], [xs_buf])
            xf_ap, xf_buf, _ = Dm["xn_f"].get(t0)
            self.collective("AllGather", (xs_ap, xs_buf), (xf_ap, xf_buf))
            if moe:
                rt, rb = self.rt_sb
                rv = rt[:, :].rearrange("p (j e) -> p j e", e=8)
                if t0 == 0:
                    ri = l // 2
                    self.dma(rv, self.I["router"][0][ri * cfg.CH:(ri + 1) * cfg.CH, :].rearrange("(j p) e -> p j e", p=P), [self.I["router"][1]], [rb])
                for tt in range(size // P):
                    pt, pb = self.psum()
                    for j in range(NCH):
                        A("pe", lambda e, pt=pt, xv=xv, rv=rv, j=j, tt=tt: e.matmul(
                            pt[:, 0:8], xv[:, j, tt * P:(tt + 1) * P], rv[:, j, :], start=(j == 0), stop=(j == NCH - 1)),
                          [xb, rb], [pb] if j == 0 else [], [] if j == 0 else [pb])
                    lt, lb = self.lgst[tt % 2]
                    A("dve", lambda e, pt=pt, lt=lt: e.tensor_copy(out=lt[:, 0:8], in_=pt[:, 0:8]), [pb], [lb])
                    self.dma(Dm["lg_in"][0][t0 + tt * P:t0 + (tt + 1) * P, :], lt[:, 0:8], [lb], (), [Dm["lg_in"][1]])

    def layer(self, l):
        cfg = self.cfg
        A = self.A
        D, T, TB, CH, NCH, KD = cfg.D, cfg.T, cfg.TB, cfg.CH, cfg.NCH, cfg.KD
        I, Dm = self.I, self.Dm
        lb = cfg.lblocks()
        gb = cfg.gblocks()
        self.phase(2)
        self.norm_phase(l, 1)
        self.phase(3)
        wvt, wvb = self.wv_sb
        wvv = wvt[:, :].rearrange("p (k m) -> p k m", m=3 * P)
        self.load_weights((I["w_v"][0][l * D:(l + 1) * D, :], I["w_v"][1]), 0, 3 * P, kchunks(D), dst=(wvv, wvb))
        uqt, uqb = self.wuq_sb
        uqv = uqt[:, :].rearrange("p (k m) -> p k m", m=256)
        self.load_weights((I["w_uq"][0][l * 512:(l + 1) * 512, :], I["w_uq"][1]), 0, 256, kchunks(512), dst=(uqv, uqb))
        ukt, ukb = self.wukv_sb
        ukv = ukt[:, :].rearrange("p (k m) -> p k m", m=256)
        self.load_weights((I["w_ukv"][0][l * 256:(l + 1) * 256, :], I["w_ukv"][1]), 0, 256, kchunks(256), dst=(ukv, ukb))
        self.phase(4)
        epi, post = self.std_epilogue(Dm["u"])

        def vhook(av, ab, bi, blk):
            t0, size = blk[0], blk[1]
            for tt in range(size // P):
                pt, pb = self.psum()
                for k in range(KD):
                    A("pe", lambda e, pt=pt, av=av, k=k, tt=tt: e.matmul(
                        pt[:, 0:3 * P], av[:, k, tt * P:(tt + 1) * P], wvv[:, k, :], start=(k == 0), stop=(k == KD - 1)),
                      [ab, wvb], [pb] if k == 0 else [], [] if k == 0 else [pb])
                vt_, vb_ = self.vst[tt % 2]
                A("act", lambda e, pt=pt, vt_=vt_: e.activation(out=vt_[:, 0:3 * P], in_=pt[:, 0:3 * P], func=AF.Copy), [pb], [vb_])
                self.dma(Dm["vtm"][0][t0 + tt * P:t0 + (tt + 1) * P, 0:3 * P], vt_[:, 0:3 * P], [vb_], (), [Dm["vtm"][1]])

        self.gemm((I["w_qkv"][0][l * D:(l + 1) * D, :], I["w_qkv"][1]), D, cfg.NQKV,
                  lambda t0, size, kch: self.load_act(Dm["xn_f"], t0, size, kch), lb, epi, post,
                  per_block=vhook)
        self.phase(5)
        epi, post = self.std_epilogue(Dm["sg"], func=AF.Sigmoid)
        gates = self.gemm_g((I["w_gate"][0][l * D:(l + 1) * D, :], I["w_gate"][1]), D, 4 * CH,
                            lambda t0, size, kch: self.load_act(Dm["xn_f"], t0, size, kch), gb, epi, post)

        def side_chain():
            yield from self.mla_prep(l)
            yield from self.rope_phase(l)

        side = side_chain()
        for _ in gates:
            for _k in range(3):
                next(side, None)
        for _ in side:
            pass
        self.phase(7)
        self.phase(7)
        self.attn_all(l)
        self.phase(8)
        for (t0_, sz_, ap_s, buf_s), (_, _, ap_f, buf_f) in zip(Dm["o_s"].items, Dm["o_f"].items):
            self.collective("AllGather", (ap_s, buf_s), (ap_f, buf_f))
        self.merge_phase(l)
        self.phase(9)
        self.resid_gemm(l)
        self.phase(10)
        self.norm_phase(l, 2)
        self.phase(11)
        self.ffn_phase(l)

    def colnorm(self, xv, xb, nch, size, nfeat, gains, outv, outb):
        A = self.A
        ones, onesb = self.ones16
        qt, qb = self.h16b
        qv = qt[:, 0:nch * 512].rearrange("p (j t) -> p j t", t=512)
        A("act", lambda e: e.activation(out=qv[:, 0:nch, 0:size], in_=xv[:, 0:nch, 0:size], func=AF.Square), [xb], [qb])
        pt, pb = self.psum()
        for j in range(nch):
            A("pe", lambda e, j=j: e.matmul(pt[:, 0:size], ones[:, :], qv[:, j, 0:size], start=(j == 0), stop=(j == nch - 1)),
              [onesb, qb], [pb] if j == 0 else [], [] if j == 0 else [pb])
        rt, rb = self.tmpD
        A("act", lambda e: e.activation(out=rt[:, 0:size], in_=pt[:, 0:size], func=AF.Sqrt, scale=1.0 / nfeat, bias=self.cfg.eps),
          [pb], [rb])
        A("dve", lambda e: e.reciprocal(out=rt[:, 0:size], in_=rt[:, 0:size]), [rb], [rb])
        for j in range(nch):
            A("dve", lambda e, j=j: e.scalar_tensor_tensor(out=outv[:, j, 0:size], in0=xv[:, j, 0:size], scalar=gains[j],
                                                           in1=rt[:, 0:size], op0=ALU.mult, op1=ALU.mult),
              [xb, rb, self.vec[1]], [outb] if j == 0 else [], [] if j == 0 else [outb])

    def mla_prep(self, l):
        cfg = self.cfg
        A = self.A
        Dm = self.Dm
        u, ub = Dm["u"]
        qk, qkb = Dm["qk"]
        uqt, uqb = self.wuq_sb
        uqv = uqt[:, :].rearrange("p (k m) -> p k m", m=256)
        ukt, ukb = self.wukv_sb
        ukv = ukt[:, :].rearrange("p (k m) -> p k m", m=256)
        for (t0, size, kind) in cfg.lblocks():
            self._mla_block(l, t0, size)
            yield
        u_ap = u
        self.dma(qk[8 * P + 64:9 * P, :], u_ap[768:832, :], [ub], (), [qkb])
        self.dma(qk[9 * P + 64:10 * P, :], u_ap[832:896, :], [ub], (), [qkb])

    def _mla_block(self, l, t0, size):
        cfg = self.cfg
        A = self.A
        Dm = self.Dm
        u, ub = Dm["u"]
        qk, qkb = Dm["qk"]
        uqt, uqb = self.wuq_sb
        uqv = uqt[:, :].rearrange("p (k m) -> p k m", m=256)
        ukt, ukb = self.wukv_sb
        ukv = ukt[:, :].rearrange("p (k m) -> p k m", m=256)
        if True:
            av, ab = self.load_act(Dm["u"], t0, size, kchunks(512), row0=0, dst=self.mla_in)
            nt, nb = self.h16
            nv = nt[:, 0:4 * 512].rearrange("p (j t) -> p j t", t=512)
            self.colnorm(av, ab, 4, size, 512, [self.vcol(l, "qng", j) for j in range(4)], nv, nb)
            ot, ob = self.rot("oh", self.obuf16)
            ov = ot[:, :].rearrange("p (j t) -> p j t", t=512)
            for mc in range(2):
                pt, pb = self.psum()
                for k in range(4):
                    A("pe", lambda e, pt=pt, k=k, mc=mc: e.matmul(pt[:, 0:size], uqv[:, k, mc * P:(mc + 1) * P], nv[:, k, 0:size],
                                                                 start=(k == 0), stop=(k == 3)),
                      [uqb, nb], [pb] if k == 0 else [], [] if k == 0 else [pb])
                A("act", lambda e, pt=pt, mc=mc: e.activation(out=ov[:, mc, 0:size], in_=pt[:, 0:size], func=AF.Copy),
                  [pb], [ob] if mc == 0 else [], [] if mc == 0 else [ob])
            self.dma(qk[0:P, t0:t0 + size], ov[:, 0, 0:size], [ob], (), [qkb])
            self.dma(qk[8 * P:8 * P + 64, t0:t0 + size], ov[0:64, 1, 0:size], [ob], (), [qkb])
            self.dma(qk[9 * P:9 * P + 64, t0:t0 + size], ov[64:128, 1, 0:size], [ob], (), [qkb])
            self._mla_block2(l, t0, size)

    def _mla_block2(self, l, t0, size):
        cfg = self.cfg
        A = self.A
        Dm = self.Dm
        qk, qkb = Dm["qk"]
        ukt, ukb = self.wukv_sb
        ukv = ukt[:, :].rearrange("p (k m) -> p k m", m=256)
        if True:
            av, ab = self.load_act(Dm["u"], t0, size, kchunks(256), row0=512, dst=self.mla_in)
            nt, nb = self.h16
            nv = nt[:, 0:4 * 512].rearrange("p (j t) -> p j t", t=512)
            self.colnorm(av, ab, 2, size, 256, [self.vcol(l, "kvng", j) for j in range(2)], nv, nb)
            ot, ob = self.rot("oh", self.obuf16)
            ov = ot[:, :].rearrange("p (j t) -> p j t", t=512)
            pt, pb = self.psum()
            for k in range(2):
                A("pe", lambda e, pt=pt, k=k: e.matmul(pt[:, 0:size], ukv[:, k, 0:P], nv[:, k, 0:size], start=(k == 0), stop=(k == 1)),
                  [ukb, nb], [pb] if k == 0 else [], [] if k == 0 else [pb])
            A("act", lambda e, pt=pt: e.activation(out=ov[:, 0, 0:size], in_=pt[:, 0:size], func=AF.Copy), [pb], [ob])
            self.dma(qk[2 * P:3 * P, t0:t0 + size], ov[:, 0, 0:size], [ob], (), [qkb])
            for tt in range(size // P):
                pt, pb = self.psum()
                for k in range(2):
                    A("pe", lambda e, pt=pt, k=k, tt=tt: e.matmul(pt[:, 0:P], nv[:, k, tt * P:(tt + 1) * P], ukv[:, k, P:2 * P],
                                                                 start=(k == 0), stop=(k == 1)),
                      [ukb, nb], [pb] if k == 0 else [], [] if k == 0 else [pb])
                A("act", lambda e, pt=pt, tt=tt: e.activation(out=ov[:, 1 + tt // 4, (tt % 4) * P:(tt % 4 + 1) * P], in_=pt[:, 0:P], func=AF.Copy),
                  [pb], (), [ob])
                self.dma(Dm["vtm"][0][t0 + tt * P:t0 + (tt + 1) * P, 3 * P:4 * P], ov[:, 1 + tt // 4, (tt % 4) * P:(tt % 4 + 1) * P],
                         [ob], (), [Dm["vtm"][1]])

    def rope_phase(self, l):
        cfg = self.cfg
        A = self.A
        Dm = self.Dm
        u, ub = Dm["u"]
        qk, qkb = Dm["qk"]
        ra, rab = self.I["rope_a"]
        jobs = [
            (qk, qkb, 8 * P, 9 * P, 0, [(0, 64, 1 * P), (64, 128, 3 * P)]),
            (u, ub, 7 * P, 8 * P, 1, [(0, 128, 4 * P)]),
            (u, ub, 9 * P, 10 * P, 1, [(0, 128, 5 * P)]),
            (u, ub, 13 * P, 14 * P, 0, [(0, 128, 10 * P)]),
            (u, ub, 15 * P, 16 * P, 0, [(0, 128, 11 * P)]),
        ]
        for (src, srcb, rx, rp, tab, outs) in jobs:
            for (t0, size, kind) in cfg.lblocks():
                xt, xb = self.h16
                pt_, pb_ = self.h16b
                self.dma(xt[:, 0:size], src[rx:rx + P, t0:t0 + size], [srcb], [xb])
                self.dma(pt_[:, 0:size], src[rp:rp + P, t0:t0 + size], [srcb], [pb_])
                ct, cb = self.tmpA
                st_, sb_ = self.tmpB
                self.dma(ct[:, 0:size], ra[(2 * tab) * P:(2 * tab + 1) * P, t0:t0 + size], [rab], [cb])
                self.dma(st_[:, 0:size], ra[(2 * tab + 1) * P:(2 * tab + 2) * P, t0:t0 + size], [rab], [sb_])
                A("dve", lambda e, ct=ct, xt=xt, size=size: e.tensor_tensor(out=ct[:, 0:size], in0=xt[:, 0:size], in1=ct[:, 0:size], op=ALU.mult),
                  [xb, cb], [cb])
                A("dve", lambda e, st_=st_, pt_=pt_, size=size: e.tensor_tensor(out=st_[:, 0:size], in0=pt_[:, 0:size], in1=st_[:, 0:size], op=ALU.mult),
                  [pb_, sb_], [sb_])
                ot, ob = self.rot("oh", self.obuf16)
                A("dve", lambda e, ot=ot, ct=ct, st_=st_, size=size: e.tensor_tensor(out=ot[:, 0:size], in0=ct[:, 0:size], in1=st_[:, 0:size], op=ALU.add),
                  [cb, sb_], [ob])
                for (p0, p1, d0) in outs:
                    self.dma(qk[d0:d0 + (p1 - p0), t0:t0 + size], ot[p0:p1, 0:size], [ob], (), [qkb])
                yield
        self.dma(qk[6 * P:7 * P, :], u[11 * P:12 * P, :], [ub], (), [qkb])
        self.dma(qk[7 * P:8 * P, :], u[12 * P:13 * P, :], [ub], (), [qkb])

    def attn_all(self, l):
        cfg = self.cfg
        A = self.A
        Dm = self.Dm
        nct = cfg.CTX // P
        nkt = cfg.TB // P
        nq = cfg.SEQ // 512
        dense = [[(kt, None) for kt in range(nkt)] for _ in range(nq)]
        ctxs = [(kt, None) for kt in range(nct)]
        et, eb = self.abuf[0]
        nsw = self.swa_mask.shape[0]
        nna = self.na_mask.shape[0]
        assert (nsw + nna) * 512 <= self.AB
        ev = et[:, 0:(nsw + nna) * 512].rearrange("p (n t) -> p n t", t=512)
        first = True
        for i in range(nsw):
            tt, tb = self.tmpA
            self.dma(tt[:, 0:512], self.I["swa_m"][0][i * P:(i + 1) * P, :], [self.I["swa_m"][1]], [tb])
            A("act", lambda e, i=i, tt=tt: e.activation(out=ev[:, i, :], in_=tt[:, 0:512], func=AF.Copy), [tb],
              [eb] if first else [], [] if first else [eb])
            first = False
        for i in range(nna):
            tt, tb = self.tmpA
            mt, mb = self.tmpB
            self.dma(tt[:, 0:512], self.I["na_b"][0][(l * nna + i) * P:(l * nna + i + 1) * P, :], [self.I["na_b"][1]], [tb])
            self.dma(mt[:, 0:512], self.I["na_m"][0][i * P:(i + 1) * P, :], [self.I["na_m"][1]], [mb])
            A("act", lambda e, tt=tt: e.activation(out=tt[:, 0:512], in_=tt[:, 0:512], func=AF.Exp), [tb], [tb])
            A("dve", lambda e, i=i, tt=tt, mt=mt: e.tensor_tensor(out=ev[:, nsw + i, :], in0=tt[:, 0:512], in1=mt[:, 0:512], op=ALU.mult),
              [tb, mb], (), [eb])
        lt, lb = self.lamv
        A("act", lambda e: e.activation(out=lt[:, 0:1], in_=self.vcol(l, "sink"), func=AF.Exp), [self.vec[1]], [lb])
        lam_init = 0.8 - 0.6 * math.exp(-0.3 * l)
        vt, vb = self.vec
        c0 = self.vcols["lam4"]
        pr, prb = self.tmpD
        for a in range(2):
            A("dve", lambda e, a=a: e.tensor_tensor(out=pr[:, a * 64:(a + 1) * 64], in0=vt[:, l, c0 + (2 * a) * 64:c0 + (2 * a + 1) * 64],
                                                    in1=vt[:, l, c0 + (2 * a + 1) * 64:c0 + (2 * a + 2) * 64], op=ALU.mult),
              [vb], [prb] if a == 0 else [], [] if a == 0 else [prb])
            A("dve", lambda e, a=a: e.reduce_sum(out=lt[:, 2 + a:3 + a], in_=pr[:, a * 64:(a + 1) * 64], axis=AX.X), [prb], (), [lb])
        A("act", lambda e: e.activation(out=lt[:, 2:4], in_=lt[:, 2:4], func=AF.Exp), [lb], (), [lb])
        A("dve", lambda e: e.tensor_tensor(out=lt[:, 4:5], in0=lt[:, 3:4], in1=lt[:, 2:3], op=ALU.subtract), [lb], (), [lb])
        A("dve", lambda e: e.tensor_scalar(out=lt[:, 4:5], in0=lt[:, 4:5], scalar1=-lam_init, scalar2=None, op0=ALU.add), [lb], (), [lb])
        A("dve", lambda e: e.tensor_scalar(out=lt[:, 5:6], in0=self.vcol(l, "subg"), scalar1=(1.0 - lam_init), scalar2=None, op0=ALU.mult),
          [vb, lb], (), [lb])

        def sched_for(kind):
            qb = []
            if kind == "swa":
                for Q in range(nq):
                    qb.append((cfg.CTX + Q * 512, 512, ctxs + [(nct + kt, None if m is None else m) for (kt, m) in self.swa_s[Q]]))
            elif kind == "na":
                for Q in range(nq):
                    qb.append((cfg.CTX + Q * 512, 512, ctxs + [(nct + kt, nsw + m) for (kt, m) in self.na_s[Q]]))
            else:
                for Q in range(nq):
                    qb.append((cfg.CTX + Q * 512, 512, dense[Q]))
            t = 0
            while t < cfg.CTX:
                s = min(512, cfg.CTX - t)
                qb.append((t, s, ctxs))
                t += s
            return qb

        mixers = [
            ("mla", [(0, 128), (1 * P, 64)], [(2 * P, 128), (3 * P, 64)], 3, 192 ** -0.5, 0),
            ("swa", [(4 * P, 128)], [(5 * P, 128)], 0, 128 ** -0.5, 1),
            ("na", [(6 * P, 128)], [(7 * P, 128)], 1, 128 ** -0.5, 2),
            ("diff", [(10 * P, 128)], [(11 * P, 128)], 2, 64 ** -0.5, 3),
        ]
        p1t, ab1 = self.abuf[1]
        A("dve", lambda e: e.memset(p1t[:, 0:8], 0.0), (), [ab1] + list(self.pslots))
        for (name, qrows, krows, vcol, scale, om) in mixers:
            self.attention(l, name, qrows, krows, vcol, scale, om, sched_for(name), ev, eb)
        A("dve", lambda e: e.memset(p1t[:, 0:8], 0.0), (), [ab1] + list(self.pslots))

    def attention(self, l, name, qrows, krows, vcol, scale, om, qblocks, ev, eb):
        cfg = self.cfg
        A = self.A
        Dm = self.Dm
        TB = cfg.TB
        nkt = TB // P
        qk, qkb = Dm["qk"]
        ones, onesb = self.ones16
        lt, lb = self.lamv
        kt_, kb = self.wbuf[0]
        nck = len(krows)
        assert nck * TB <= self.WB and nkt * P <= self.WB
        kv = kt_[:, 0:nck * TB].rearrange("p (c t) -> p c t", t=TB)
        for ci, (r0, nr) in enumerate(krows):
            self.dma(kv[0:nr, ci, :], qk[r0:r0 + nr, :], [qkb], [kb] if ci == 0 else [], [] if ci == 0 else [kb])
        vt_, vb_ = self.wbuf[1]
        vv = vt_[:, 0:nkt * P].rearrange("p (n d) -> p n d", d=P)
        self.dma(vv, Dm["vtm"][0][:, vcol * P:(vcol + 1) * P].rearrange("(n p) d -> p n d", p=P), [Dm["vtm"][1]], [vb_])
        diff = (name == "diff")
        nmaps = 2 if diff else 1
        ACC = (0, 1, 2, 3)
        SP_ = (4, 5, 6, 7)
        for (q0, qs, klist) in qblocks:
            self._attn_qblock(name, qrows, krows, scale, om, q0, qs, klist, ev, eb, kv, kb, vv, vb_, nck, diff, nmaps)

    def _attn_qblock(self, name, qrows, krows, scale, om, q0, qs, klist, ev, eb, kv, kb, vv, vb_, nck, diff, nmaps):
        cfg = self.cfg
        A = self.A
        Dm = self.Dm
        qk, qkb = Dm["qk"]
        ones, onesb = self.ones16
        lt, lb = self.lamv
        ACC = (0, 1, 2, 3)
        SP_ = (4, 5, 6, 7)
        if True:
            qt_, qb_ = self.rot("oh", self.obuf16)
            qv = qt_[:, :].rearrange("p (j t) -> p j t", t=512)
            for ci, (r0, nr) in enumerate(qrows):
                self.dma(qv[0:nr, ci, 0:qs], qk[r0:r0 + nr, q0:q0 + qs], [qkb], [qb_] if ci == 0 else [], [] if ci == 0 else [qb_])
            accs = [(self.psum(ACC), self.psum(ACC)) for _ in range(nmaps)]
            nk = len(klist)
            pt_, ab1 = self.abuf[1]
            jobs = [(ki, kt, etile, mp) for ki, (kt, etile) in enumerate(klist) for mp in range(nmaps)]

            def emit_s(job):
                ki, kt, etile, mp = job
                ps, psb = self.psum(SP_)
                if diff:
                    A("pe", lambda e: e.matmul(ps[:, 0:qs], kv[mp * 64:(mp + 1) * 64, 0, kt * P:(kt + 1) * P],
                                               qv[mp * 64:(mp + 1) * 64, 0, 0:qs], start=True, stop=True),
                      [kb, qb_], [psb])
                else:
                    for ci, (r0, nr) in enumerate(krows):
                        A("pe", lambda e, ci=ci, nr=nr: e.matmul(ps[:, 0:qs], kv[0:nr, ci, kt * P:(kt + 1) * P],
                                                                 qv[0:nr, ci, 0:qs], start=(ci == 0), stop=(ci == nck - 1)),
                          [kb, qb_], [psb] if ci == 0 else [], [] if ci == 0 else [psb])
                return ps, psb

            def emit_rest(job, ps, psb):
                ki, kt, etile, mp = job
                (po, pob), (pd, pdb) = accs[mp]
                slot = (self.cnt["oh"] % 8)
                self.cnt["oh"] += 1
                pbuf = self.pslots[slot]
                pv = pt_[:, slot * 512:(slot + 1) * 512]
                A("act", lambda e: e.activation(out=pv[:, 0:qs], in_=ps[:, 0:qs], func=AF.Exp, scale=scale), [psb], [pbuf])
                if etile is not None:
                    A("dve", lambda e: e.tensor_tensor(out=pv[:, 0:qs], in0=pv[:, 0:qs], in1=ev[:, etile, 0:qs], op=ALU.mult),
                      [pbuf, eb], [pbuf])
                A("pe", lambda e: e.matmul(po[:, 0:qs], vv[:, kt, :], pv[:, 0:qs], start=(ki == 0), stop=(ki == nk - 1)),
                  [vb_, pbuf], [pob] if ki == 0 else [], [] if ki == 0 else [pob])
                A("pe", lambda e: e.matmul(pd[:, 0:qs], ones[:, :], pv[:, 0:qs], start=(ki == 0), stop=(ki == nk - 1)),
                  [onesb, pbuf], [pdb] if ki == 0 else [], [] if ki == 0 else [pdb])

            LA = 2
            pend = []
            for job in jobs:
                pend.append((job,) + emit_s(job))
                if len(pend) > LA:
                    emit_rest(*pend.pop(0))
            while pend:
                emit_rest(*pend.pop(0))
            res = []
            for mp in range(nmaps):
                (po, pob), (pd, pdb) = accs[mp]
                rt, rb = self.tmpA if mp == 0 else self.tmpB
                if name == "swa":
                    A("dve", lambda e, rt=rt, pd=pd: e.tensor_scalar(out=rt[:, 0:qs], in0=pd[:, 0:qs], scalar1=lt[:, 0:1], scalar2=None, op0=ALU.add),
                      [pdb, lb], [rb])
                    A("dve", lambda e, rt=rt: e.reciprocal(out=rt[:, 0:qs], in_=rt[:, 0:qs]), [rb], [rb])
                else:
                    A("dve", lambda e, rt=rt, pd=pd: e.reciprocal(out=rt[:, 0:qs], in_=pd[:, 0:qs]), [pdb], [rb])
                A("dve", lambda e, rt=rt, po=po: e.tensor_tensor(out=rt[:, 0:qs], in0=po[:, 0:qs], in1=rt[:, 0:qs], op=ALU.mult), [pob, rb], [rb])
                res.append((rt, rb))
            ot, ob = self.rot("oh", self.obuf16)
            if not diff:
                rt, rb = res[0]
                A("act", lambda e, ot=ot, rt=rt: e.activation(out=ot[:, 0:qs], in_=rt[:, 0:qs], func=AF.Copy), [rb], [ob])
            else:
                (r1, r1b), (r2, r2b) = res
                A("dve", lambda e, r1=r1, r2=r2: e.scalar_tensor_tensor(out=r1[:, 0:qs], in0=r2[:, 0:qs], scalar=lt[:, 4:5], in1=r1[:, 0:qs],
                                                                        op0=ALU.mult, op1=ALU.add), [r1b, r2b, lb], [r1b])
                qq, qqb = self.h16
                A("act", lambda e, qq=qq, r1=r1: e.activation(out=qq[:, 0:qs], in_=r1[:, 0:qs], func=AF.Square), [r1b], [qqb])
                pn, pnb = self.psum(SP_)
                A("pe", lambda e, pn=pn, qq=qq: e.matmul(pn[:, 0:qs], ones[:, :], qq[:, 0:qs], start=True, stop=True), [onesb, qqb], [pnb])
                A("act", lambda e, r2=r2, pn=pn: e.activation(out=r2[:, 0:qs], in_=pn[:, 0:qs], func=AF.Sqrt, scale=1.0 / 128, bias=cfg.eps), [pnb], [r2b])
                A("dve", lambda e, r2=r2: e.reciprocal(out=r2[:, 0:qs], in_=r2[:, 0:qs]), [r2b], [r2b])
                A("dve", lambda e, ot=ot, r1=r1, r2=r2: e.scalar_tensor_tensor(out=ot[:, 0:qs], in0=r1[:, 0:qs], scalar=lt[:, 5:6], in1=r2[:, 0:qs],
                                                                               op0=ALU.mult, op1=ALU.mult), [r1b, r2b, lb], [ob])
            os_ap, os_buf, ob0 = Dm["o_s"].get(q0)
            self.dma(os_ap[om * P:(om + 1) * P, q0 - ob0:q0 - ob0 + qs], ot[:, 0:qs], [ob], (), [os_buf])

    def merge_phase(self, l):
        cfg = self.cfg
        A = self.A
        Dm, I = self.Dm, self.I
        D, CH, NCH, TB = cfg.D, cfg.CH, cfg.NCH, cfg.TB
        wv, wbl_ = self.load_weights((I["w_br"][0][l * 2048:(l + 1) * 2048, :], I["w_br"][1]), 0, CH, kchunks(2048))
        wb = wbl_[0]
        for (t0, size, kind) in cfg.lblocks():
            self._merge_block(0, t0, size, wv, wb)

    def _merge_block(self, b, t0, size, wv, wb):
        cfg = self.cfg
        A = self.A
        Dm = self.Dm
        NCH = cfg.NCH
        av, ab = self.load_act(Dm["o_f"], t0, size, kchunks(2048), row0=0)
        ot, ob = self.h16b
        ov = ot[:, 0:NCH * 512].rearrange("p (j t) -> p j t", t=512)
        acc, accb = self.tmpA
        accv = acc[:, 0:NCH * 512].rearrange("p (j t) -> p j t", t=512)
        for mc in range(NCH):
            self._merge_mc(mc, t0, size, wv, wb, av, ab, accv, accb)
        A("act", lambda e: e.activation(out=ov[:, :, 0:size], in_=accv[:, :, 0:size], func=AF.Copy), [accb], [ob])
        ms_ap, ms_buf, mb0 = Dm["mg_s"].get(t0)
        self.dma(ms_ap[:, t0 - mb0:t0 - mb0 + size].rearrange("(j p) t -> p j t", p=P), ov[:, :, 0:size], [ob], [ms_buf])
        mf_ap, mf_buf, _ = Dm["mg_f"].get(t0)
        self.collective("AllGather", (ms_ap, ms_buf), (mf_ap, mf_buf))

    def _merge_mc(self, mc, t0, size, wv, wb, av, ab, accv, accb):
        cfg = self.cfg
        A = self.A
        Dm = self.Dm
        NCH = cfg.NCH
        gt, gb_ = self.rot("oh", self.obuf16)
        gv = gt[:, :].rearrange("p (j t) -> p j t", t=512)
        for m in range(4):
            r0 = (m * NCH + mc) * P
            self.dma(gv[:, m, 0:size], Dm["sg"][0][r0:r0 + P, t0:t0 + size], [Dm["sg"][1]], [gb_] if m == 0 else [], [] if m == 0 else [gb_])
        tmp, tmpb = self.tmpB
        for m in range(4):
            pt, pb = self.psum()
            for ih in range(4):
                k = ih * 4 + m
                A("pe", lambda e, pt=pt, k=k, ih=ih: e.matmul(pt[:, 0:size], wv[:, k, mc * P:(mc + 1) * P], av[:, k, 0:size],
                                                             start=(ih == 0), stop=(ih == 3)),
                  [wb, ab], [pb] if ih == 0 else [], [] if ih == 0 else [pb])
            if m == 0:
                A("dve", lambda e, pt=pt, m=m: e.tensor_tensor(out=accv[:, mc, 0:size], in0=pt[:, 0:size], in1=gv[:, m, 0:size], op=ALU.mult),
                  [pb, gb_], [accb] if mc == 0 else [], [] if mc == 0 else [accb])
            else:
                A("dve", lambda e, pt=pt, m=m: e.tensor_tensor(out=tmp[:, 0:size], in0=pt[:, 0:size], in1=gv[:, m, 0:size], op=ALU.mult),
                  [pb, gb_], [tmpb])
                A("dve", lambda e: e.tensor_tensor(out=accv[:, mc, 0:size], in0=accv[:, mc, 0:size], in1=tmp[:, 0:size], op=ALU.add),
                  [tmpb, accb], (), [accb])

    def resid_gemm(self, l):
        cfg = self.cfg
        A = self.A
        Dm, I = self.Dm, self.I
        D, CH, NCH = cfg.D, cfg.CH, cfg.NCH
        hT, hTb = Dm["hT"]
        state = {}

        def epi(ps, pb, gi, j, m0, msz, bi, blk):
            t0, size, r = blk
            if j == 0:
                ht, hb = self.tmpA if bi % 2 == 0 else self.tmpB
                hv = ht[:, 0:NCH * 512].rearrange("p (j t) -> p j t", t=512)
                self.dma(hv[:, :, 0:size], hT[:, t0:t0 + size].rearrange("(j p) t -> p j t", p=P), [hTb], [hb])
                state["h"] = (hv, hb)
            hv, hb = state["h"]
            g = self.mod(l, 2, j, r)
            A("dve", lambda e: e.scalar_tensor_tensor(out=hv[:, j, 0:size], in0=ps, scalar=g, in1=hv[:, j, 0:size], op0=ALU.mult, op1=ALU.add),
              [pb, self.modv[1]], (), [hb])

        def post(gi, g0, mg, bi, blk):
            t0, size, r = blk
            hv, hb = state["h"]
            self.dma(hT[:, t0:t0 + size].rearrange("(j p) t -> p j t", p=P), hv[:, :, 0:size], [hb], (), [hTb])

        self.gemm((I["w_out"][0][l * D:(l + 1) * D, :], I["w_out"][1]), D, CH,
                  lambda t0, size, kch: self.load_act(Dm["mg_f"], t0, size, kch), cfg.gblocks(), epi, post)

    def ffn_phase(self, l):
        cfg = self.cfg
        A = self.A
        Dm, I = self.Dm, self.I
        D, CH, NCH, T = cfg.D, cfg.CH, cfg.NCH, cfg.T
        moe = (l % 2 == 1)
        i = l // 2
        if moe:
            F = cfg.FE
            self.moe_gates(i)
            for k in range(2):
                W13 = (I["m_w13"][0][(2 * i + k) * D:(2 * i + k + 1) * D, :], I["m_w13"][1])
                W2 = (I["m_w2"][0][(2 * i + k) * F:(2 * i + k + 1) * F, :], I["m_w2"][1])
                self.ffn_core(W13, W2, F, gate_row=k, accumulate=(k == 1))
        else:
            F = cfg.FD
            W13 = (I["f_w13"][0][i * D:(i + 1) * D, :], I["f_w13"][1])
            W2 = (I["f_w2"][0][i * F:(i + 1) * F, :], I["f_w2"][1])
            self.ffn_core(W13, W2, F, gate_row=None, accumulate=False)
        for (t0_, sz_, ap_s, buf_s), (_, _, ap_f, buf_f) in zip(Dm["y_in"].items, Dm["y_out"].items):
            self.collective("ReduceScatter", (ap_s, buf_s), (ap_f, buf_f))
        hT, hTb = Dm["hT"]
        for bi, (t0, size, r) in enumerate(cfg.gblocks()):
            ht, hb = self.tmpA if bi % 2 == 0 else self.tmpB
            hv = ht[:, 0:NCH * 512].rearrange("p (j t) -> p j t", t=512)
            yt, yb = self.tmpC
            yv = yt[:, 0:NCH * 512].rearrange("p (j t) -> p j t", t=512)
            self.dma(hv[:, :, 0:size], hT[:, t0:t0 + size].rearrange("(j p) t -> p j t", p=P), [hTb], [hb])
            yo_ap, yo_buf, yb0 = Dm["y_out"].get(t0)
            self.dma(yv[:, :, 0:size], yo_ap[:, t0 - yb0:t0 - yb0 + size].rearrange("(j p) t -> p j t", p=P), [yo_buf], [yb])
            for j in range(NCH):
                g = self.mod(l, 5, j, r)
                A("dve", lambda e, hv=hv, yv=yv, j=j, g=g, size=size: e.scalar_tensor_tensor(
                    out=hv[:, j, 0:size], in0=yv[:, j, 0:size], scalar=g, in1=hv[:, j, 0:size], op0=ALU.mult, op1=ALU.add),
                  [yb, hb, self.modv[1]], (), [hb])
            self.dma(hT[:, t0:t0 + size].rearrange("(j p) t -> p j t", p=P), hv[:, :, 0:size], [hb], (), [hTb])

    def ffn_core(self, W13, W2, F, gate_row, accumulate):
        cfg = self.cfg
        A = self.A
        Dm = self.Dm
        D = cfg.D
        gb = cfg.gblocks()
        hid, hidb = Dm["hid"]
        state = {}

        def epi(ps, pb, gi, j, m0, msz, bi, blk):
            t0, size, r = blk
            if j == 0:
                state["o"] = self.rot("oh", self.obuf16)
            ot, ob = state["o"]
            ov = ot[:, :].rearrange("p (j t) -> p j t", t=512)
            st_, sb_ = self.h16
            if j % 2 == 0:
                A("act", lambda e: e.activation(out=st_[:, 0:size], in_=ps, func=AF.Silu), [pb], [sb_])
            else:
                A("dve", lambda e: e.tensor_tensor(out=ov[:, j // 2, 0:size], in0=ps, in1=st_[:, 0:size], op=ALU.mult),
                  [pb, sb_], [ob] if j == 1 else [], [] if j == 1 else [ob])

        def post(gi, g0, mg, bi, blk):
            t0, size, r = blk
            ot, ob = state["o"]
            ov = ot[:, :].rearrange("p (j t) -> p j t", t=512)
            nj = mg // (2 * P)
            f0 = g0 // 2
            self.dma(hid[f0:f0 + nj * P, t0:t0 + size].rearrange("(j p) t -> p j t", p=P), ov[:, 0:nj, 0:size], [ob], (), [hidb])

        self.gemm(W13, D, 2 * F, lambda t0, size, kch: self.load_act(Dm["xn_f"], t0, size, kch), gb, epi, post)
        kch = kchunks(F)
        nkc = len(kch)
        tw = 512 if nkc * 512 <= self.AB else 256
        blocks2 = []
        for (t0, size, r) in gb:
            t = t0
            while t < t0 + size:
                s = min(tw, t0 + size - t)
                blocks2.append((t, s, r))
                t += s
        st2 = {}

        def epi2(ps, pb, gi, j, m0, msz, bi, blk):
            t0, size, r = blk
            if j == 0:
                st2["o"] = self.rot("o", self.obuf)
                st2["g0"] = m0
                if gate_row is not None:
                    gt, gtb = self.tmpD
                    self.dma(gt[:, 0:size], Dm["gvec"][0][gate_row:gate_row + 1, t0:t0 + size].partition_broadcast(P), [Dm["gvec"][1]], [gtb])
                    st2["g"] = (gt, gtb)
                if accumulate:
                    pt_, ptb = self.tmpC
                    pv = pt_[:, :].rearrange("p (j t) -> p j t", t=512)
                    st2["p"] = (pv, ptb)
            ot, ob = st2["o"]
            ov = ot[:, :].rearrange("p (j t) -> p j t", t=512)
            if accumulate:
                pv, ptb = st2["p"]
                g0_ = m0
                y_in, y_inb, yb0 = Dm["y_in"].get(t0)
                self.dma(pv[0:msz, j, 0:size], y_in[g0_:g0_ + msz, t0 - yb0:t0 - yb0 + size], [y_inb], [ptb] if j == 0 else [], [] if j == 0 else [ptb])
            if gate_row is not None:
                gt, gtb = st2["g"]
                A("dve", lambda e: e.tensor_tensor(out=ov[0:msz, j, 0:size], in0=ps, in1=gt[0:msz, 0:size], op=ALU.mult),
                  [pb, gtb], [ob] if j == 0 else [], [] if j == 0 else [ob])
                if accumulate:
                    A("dve", lambda e: e.tensor_tensor(out=ov[0:msz, j, 0:size], in0=ov[0:msz, j, 0:size], in1=pv[0:msz, j, 0:size], op=ALU.add),
                      [ptb, ob], (), [ob])
            else:
                A("act", lambda e: e.activation(out=ov[0:msz, j, 0:size], in_=ps, func=AF.Copy),
                  [pb], [ob] if j == 0 else [], [] if j == 0 else [ob])

        def post2(gi, g0, mg, bi, blk):
            t0, size, r = blk
            ot, ob = st2["o"]
            ov = ot[:, :].rearrange("p (j t) -> p j t", t=512)
            nj = mg // P
            y_in, y_inb, yb0 = Dm["y_in"].get(t0)
            self.dma(y_in[g0:g0 + mg, t0 - yb0:t0 - yb0 + size].rearrange("(j p) t -> p j t", p=P), ov[:, 0:nj, 0:size], [ob], (), [y_inb])

        self.gemm(W2, F, D, lambda t0, size, kch: self.load_act(Dm["hid"], t0, size, kch), blocks2, epi2, post2, mg_max=512, combined=(F > 2048 or bool(os.environ.get("MK_FORCE_COMBINED"))))

    def moe_gates(self, i):
        cfg = self.cfg
        A = self.A
        Dm, I = self.Dm, self.I
        T = cfg.T
        n = T // P
        assert n * 8 <= 4 * 512 and 8 * n <= 4 * 512
        self.collective("AllReduce", Dm["lg_in"], Dm["lg_out"])
        lg, lgb = self.tmpA
        lv = lg[:, 0:n * 8].rearrange("p (n e) -> p n e", e=8)
        self.dma(lv, Dm["lg_out"][0].rearrange("(n p) e -> p n e", p=P), [Dm["lg_out"][1]], [lgb])
        w, wb = self.tmpB
        mk, mkb = self.tmpC
        mv = mk[:, 0:n * 8].rearrange("p (n e) -> p n e", e=8)
        m1 = w[:, 0:n]
        m2 = w[:, n:2 * n]
        g1 = w[:, 2 * n:3 * n]
        g2 = w[:, 3 * n:4 * n]
        accs = [w[:, 4 * n:5 * n], w[:, 7 * n:8 * n]]
        t1 = w[:, 5 * n:6 * n]
        t2 = w[:, 6 * n:7 * n]
        A("dve", lambda e: e.tensor_reduce(out=m1, in_=lv, axis=AX.X, op=ALU.max), [lgb], [wb])
        for e_ in range(8):
            A("dve", lambda e, e_=e_: e.tensor_tensor(out=t1, in0=lv[:, :, e_], in1=m1, op=ALU.is_equal), [lgb, wb], (), [wb])
            A("dve", lambda e, e_=e_: e.scalar_tensor_tensor(out=mv[:, :, e_], in0=t1, scalar=-1e30, in1=lv[:, :, e_], op0=ALU.mult, op1=ALU.add),
              [lgb, wb], [mkb] if e_ == 0 else [], [] if e_ == 0 else [mkb])
        A("dve", lambda e: e.tensor_reduce(out=m2, in_=mv, axis=AX.X, op=ALU.max), [mkb, wb], (), [wb])
        A("dve", lambda e: e.tensor_tensor(out=t1, in0=m2, in1=m1, op=ALU.subtract), [wb], (), [wb])
        A("act", lambda e: e.activation(out=t1, in_=t1, func=AF.Exp), [wb], (), [wb])
        A("dve", lambda e: e.tensor_scalar(out=t2, in0=t1, scalar1=1.0, scalar2=None, op0=ALU.add), [wb], (), [wb])
        A("dve", lambda e: e.reciprocal(out=g1, in_=t2), [wb], (), [wb])
        A("dve", lambda e: e.tensor_tensor(out=g2, in0=t1, in1=g1, op=ALU.mult), [wb], (), [wb])
        sel0 = self.vcols["sel"]
        vt, vb = self.vec
        for e_ in range(8):
            A("dve", lambda e, e_=e_: e.tensor_tensor(out=t1, in0=lv[:, :, e_], in1=m1, op=ALU.is_equal), [lgb, wb], (), [wb])
            A("dve", lambda e: e.tensor_tensor(out=t1, in0=t1, in1=g1, op=ALU.mult), [wb], (), [wb])
            A("dve", lambda e, e_=e_: e.tensor_tensor(out=t2, in0=mv[:, :, e_], in1=m2, op=ALU.is_equal), [mkb, wb], (), [wb])
            A("dve", lambda e: e.tensor_tensor(out=t2, in0=t2, in1=g2, op=ALU.mult), [wb], (), [wb])
            A("dve", lambda e: e.tensor_tensor(out=t1, in0=t1, in1=t2, op=ALU.add), [wb], (), [wb])
            for k in range(2):
                s = vt[:, 0, sel0 + 8 * k + e_:sel0 + 8 * k + e_ + 1]
                acc = accs[k]
                if e_ == 0:
                    A("dve", lambda e, s=s, acc=acc: e.tensor_scalar(out=acc, in0=t1, scalar1=s, scalar2=None, op0=ALU.mult), [wb, vb], (), [wb])
                else:
                    A("dve", lambda e, s=s, acc=acc: e.scalar_tensor_tensor(out=acc, in0=t1, scalar=s, in1=acc, op0=ALU.mult, op1=ALU.add), [wb, vb], (), [wb])
        first = True
        for k in range(2):
            gvd = Dm["gvec"][0][k, :].rearrange("(n p) -> p n", p=P)
            acc = accs[k]
            for n0 in range(0, n, 16):
                n1 = min(n, n0 + 16)
                self.A("sp", lambda e, n0=n0, n1=n1, gvd=gvd, acc=acc: e.dma_start(out=gvd[:, n0:n1], in_=acc[:, n0:n1], allow_slow_non_contiguous=True),
                       [wb], [Dm["gvec"][1]] if first else [], [] if first else [Dm["gvec"][1]], dma=True)
                first = False


IN_SIZES = (512, 256, 64, 512, 256, 256, 512, 512, 512, 512, 512, 512)


def rope_perm(dim):
    q = dim // 4
    return np.concatenate([np.arange(q, 2 * q), np.arange(0, q), np.arange(3 * q, 4 * q), np.arange(2 * q, 3 * q)])


def rope_tables(cfg, dim):
    n = cfg.SEQ
    pos = np.arange(n, dtype=np.int32)
    row = (pos // cfg.GW).astype(np.float32)
    col = (pos % cfg.GW).astype(np.float32)
    quarter = dim // 4
    inv_freq = (np.float32(10000.0) ** (-np.arange(quarter, dtype=np.float32) / np.float32(quarter))).astype(np.float32)
    ang_r = row[:, None] * inv_freq
    ang_c = col[:, None] * inv_freq
    ang = np.concatenate([ang_r, ang_r, ang_c, ang_c], axis=-1).astype(np.float32)
    cos = np.cos(ang).astype(np.float32)
    sin = np.sin(ang).astype(np.float32)
    sign = np.concatenate([-np.ones(quarter), np.ones(quarter), -np.ones(quarter), np.ones(quarter)]).astype(np.float32)
    sin = sin * sign[None, :]
    cosT = np.ones((dim, cfg.TB), np.float32)
    sinT = np.zeros((dim, cfg.TB), np.float32)
    cosT[:, cfg.CTX:] = cos.T
    sinT[:, cfg.CTX:] = sin.T
    return cosT, sinT


def prep_inputs(cfg, B, inp):
    L, D, CH, NCH, TB, T = cfg.DEPTH, cfg.D, cfg.CH, cfg.NCH, cfg.TB, cfg.T
    f = lambda a: np.ascontiguousarray(np.asarray(a, dtype=np.float32))
    x, c, ctx, c_ctx = f(inp["x"]), f(inp["c"]), f(inp["ctx"]), f(inp["c_ctx"])
    w_in = f(inp["w_in"])
    off = np.cumsum((0,) + IN_SIZES)
    gate0 = off[-1]
    cos64, sin64 = rope_tables(cfg, 64)
    cos128, sin128 = rope_tables(cfg, 128)
    rope_a = np.concatenate([np.tile(cos64, (2, 1)), np.tile(sin64, (2, 1)), cos128, sin128], axis=0)
    p64 = rope_perm(64)
    p128 = rope_perm(128)
    p64x2 = np.concatenate([p64, 64 + p64])
    nna = B.na_mask.shape[0]
    vcols, NV = B.vcols, B.NV
    mod_w = f(inp["mod_w"])
    shared = {}
    for i in range(G):
        ch = slice(i * CH, (i + 1) * CH)
        m = {}
        vecs = np.zeros((L, P, NV), np.float32)
        for l in range(L):
            for j in range(NCH):
                sl = slice(i * CH + j * P, i * CH + (j + 1) * P)
                vecs[l, :, vcols["n1g"] + j] = inp["norm1_g"][l][sl]
                vecs[l, :, vcols["n2g"] + j] = inp["norm2_g"][l][sl]
                vecs[l, :, vcols["fng"] + j] = inp["final_norm_g"][sl]
                for part in range(6):
                    vecs[l, :, vcols["modb"] + part * NCH + j] = inp["mod_b"][l][part * D + i * CH + j * P: part * D + i * CH + (j + 1) * P]
            for j in range(4):
                vecs[l, :, vcols["qng"] + j] = inp["mla_q_norm"][l][j * P:(j + 1) * P]
            for j in range(2):
                vecs[l, :, vcols["kvng"] + j] = inp["mla_kv_norm"][l][j * P:(j + 1) * P]
            vecs[l, :, vcols["sink"]] = inp["swa_sink"][l][i]
            vecs[l, :, vcols["subg"]] = inp["diff_subln_g"][l]
            vecs[l, :, vcols["lam4"]:vcols["lam4"] + 256] = np.asarray(inp["diff_lambda"][l]).reshape(1, 256)
            vecs[l, :, vcols["sel"] + 2 * i] = 1.0
            vecs[l, :, vcols["sel"] + 8 + 2 * i + 1] = 1.0
        m["vecs"] = vecs.reshape(L * P, NV)
        m["mod_w"] = np.concatenate([mod_w[:, :, part * D + i * CH: part * D + (i + 1) * CH] for part in range(6)], axis=2).reshape(L * D, 6 * CH)
        m["w_gate"] = np.concatenate([w_in[:, :, gate0 + mm * D + i * CH: gate0 + mm * D + (i + 1) * CH] for mm in range(4)], axis=2).reshape(L * D, 4 * CH)
        cq = w_in[:, :, off[0]:off[1]]
        ckv = w_in[:, :, off[1]:off[2]]
        kr = w_in[:, :, off[2]:off[3]]
        sq = w_in[:, :, off[3] + i * P: off[3] + (i + 1) * P]
        sk = w_in[:, :, off[4] + (i // 2) * P: off[4] + (i // 2 + 1) * P]
        sv = w_in[:, :, off[5] + (i // 2) * P: off[5] + (i // 2 + 1) * P]
        nq_ = w_in[:, :, off[6] + i * P: off[6] + (i + 1) * P]
        nk_ = w_in[:, :, off[7] + i * P: off[7] + (i + 1) * P]
        nv_ = w_in[:, :, off[8] + i * P: off[8] + (i + 1) * P]
        dq = w_in[:, :, off[9] + i * P: off[9] + (i + 1) * P]
        dk = w_in[:, :, off[10] + i * P: off[10] + (i + 1) * P]
        dv = w_in[:, :, off[11] + i * P: off[11] + (i + 1) * P]
        m["w_qkv"] = np.concatenate([cq, ckv, kr, kr[:, :, p64], sq, sq[:, :, p128], sk, sk[:, :, p128], nq_, nk_,
                                     dq, dq[:, :, p64x2], dk, dk[:, :, p64x2]], axis=2).reshape(L * D, cfg.NQKV)
        m["w_v"] = np.concatenate([sv, nv_, dv], axis=2).reshape(L * D, 3 * P)
        wuq = f(inp["mla_w_uq"])[:, :, i * 192:(i + 1) * 192]
        m["w_uq"] = np.concatenate([wuq[:, :, 0:128], wuq[:, :, 128:192], wuq[:, :, 128:192][:, :, p64]], axis=2).reshape(L * 512, 256)
        m["w_ukv"] = np.ascontiguousarray(f(inp["mla_w_ukv"])[:, :, i * 256:(i + 1) * 256]).reshape(L * 256, 256)
        wbr = f(inp["w_branch"])[:, :, :, ch]
        wbr = wbr.reshape(L, 4, 4, P, CH).transpose(0, 2, 1, 3, 4)
        m["w_br"] = np.ascontiguousarray(wbr).reshape(L * 2048, CH)
        m["w_out"] = np.ascontiguousarray(f(inp["w_out"])[:, :, ch]).reshape(L * D, CH)
        FDr = cfg.DFF // G
        w1 = np.zeros((cfg.ND, D, cfg.FD), np.float32)
        w3 = np.zeros((cfg.ND, D, cfg.FD), np.float32)
        w2 = np.zeros((cfg.ND, cfg.FD, D), np.float32)
        w1[:, :, :FDr] = inp["ffn_w1"][:, :, i * FDr:(i + 1) * FDr]
        w3[:, :, :FDr] = inp["ffn_w3"][:, :, i * FDr:(i + 1) * FDr]
        w2[:, :FDr, :] = inp["ffn_w2"][:, i * FDr:(i + 1) * FDr, :]
        w13 = np.stack([w1.reshape(cfg.ND, D, cfg.FD // P, P), w3.reshape(cfg.ND, D, cfg.FD // P, P)], axis=3)
        m["f_w13"] = w13.reshape(cfg.ND * D, 2 * cfg.FD)
        m["f_w2"] = w2.reshape(cfg.ND * cfg.FD, D)
        if cfg.NM:
            e1 = np.zeros((cfg.NM, 2, D, cfg.FE), np.float32)
            e3 = np.zeros((cfg.NM, 2, D, cfg.FE), np.float32)
            e2 = np.zeros((cfg.NM, 2, cfg.FE, D), np.float32)
            e1[:, :, :, :cfg.DFF] = inp["moe_w1"][:, 2 * i:2 * i + 2]
            e3[:, :, :, :cfg.DFF] = inp["moe_w3"][:, 2 * i:2 * i + 2]
            e2[:, :, :cfg.DFF, :] = inp["moe_w2"][:, 2 * i:2 * i + 2]
            e13 = np.stack([e1.reshape(cfg.NM, 2, D, cfg.FE // P, P), e3.reshape(cfg.NM, 2, D, cfg.FE // P, P)], axis=4)
            m["m_w13"] = e13.reshape(cfg.NM * 2 * D, 2 * cfg.FE)
            m["m_w2"] = e2.reshape(cfg.NM * 2 * cfg.FE, D)
            m["router"] = np.ascontiguousarray(f(inp["moe_router"])[:, ch, :]).reshape(cfg.NM * CH, 8)
        m["rope_a"] = rope_a
        m["swa_m"] = B.swa_mask.reshape(-1, 512)
        m["na_m"] = B.na_mask.reshape(-1, 512)
        rpb = f(inp["na_rpb"])
        m["na_b"] = np.ascontiguousarray(rpb[:, i][:, B.na_dr, B.na_dc]).reshape(L * nna * P, 512)
        shared[i] = m
    maps = []
    for core in range(NCORES):
        g, i = core // G, core % G
        m = dict(shared[i])
        tok = np.concatenate([ctx[g], x[g]], axis=0)
        m["xT"] = np.ascontiguousarray(tok[:, i * CH:(i + 1) * CH].T)
        m["cT"] = np.ascontiguousarray(np.stack([c[g], c_ctx], axis=1))
        maps.append(m)
    return maps


_CACHE = {}


def run(cfg, inputs, debug=(), stop=999):
    key = (cfg.D, cfg.SEQ, cfg.CTX, cfg.DFF, cfg.DEPTH, tuple(debug), stop)
    if key not in _CACHE:
        B = Builder(cfg, debug=debug, stop=stop)
        B.pslots = [Buf(f"pslot{i}") for i in range(8)]
        B.build()
        _CACHE[key] = B
    B = _CACHE[key]
    maps = prep_inputs(cfg, B, inputs)
    for m in maps:
        for k, (shape, dt) in B.inputs.items():
            assert tuple(m[k].shape) == shape, (k, m[k].shape, shape)
            if m[k].dtype != np.float32 or not m[k].flags["C_CONTIGUOUS"]:
                m[k] = np.ascontiguousarray(m[k], dtype=np.float32)
    res = run_bass_kernel_spmd(B.nc, maps, core_ids=list(range(NCORES)))
    if stop < 999:
        return None, res
    TB, CTX = cfg.TB, cfg.CTX
    outs = []
    for g in range(2):
        outT = np.concatenate([res.results[g * G + i]["outT"] for i in range(G)], axis=0)
        outs.append(outT.T[CTX:TB])
    out = np.stack(outs, axis=0)
    return np.ascontiguousarray(out.astype(np.float32)), res


def kernel(**inputs):
    cfg = Cfg()
    out, _ = run(cfg, inputs)
    return out
```
